# Optimizing a Trainium2 kernel written in Bass

```python
import math
import jax
import jax.numpy as jnp
from jax import lax
import numpy as np

D_MODEL = 1024
BATCH = 2
SEQ = 16384
DEPTH = 4

CTX_LEN = 256
GRID_W = 64
N_MIXERS = 3
N_A = (DEPTH + 2) // 3
N_B = (DEPTH + 1) // 3
N_C = DEPTH // 3
EPS = 1e-6
N_MOD = 6

DN_HEADS = 8
DN_DK = D_MODEL // DN_HEADS
DN_DV = D_MODEL // DN_HEADS
DN_CHUNK = 64
DN_IN = 4 * D_MODEL + 4 * DN_HEADS

HY_ORDER = 2
HY_BANDS = 16
HY_EMB = 1 + 2 * HY_BANDS
HY_HIDDEN = 64
HY_TARGET = 1e-2
HY_FAST = 0.3
HY_SLOW = 1.5

N_EXPERTS = 32
N_GROUPS = 8
EXPERTS_PER_GROUP = N_EXPERTS // N_GROUPS
GROUP_SCORE_K = 2
TOP_K = 2
D_EXPERT = 512
MOE_BLOCK = 512

kernel_name = 'hybrid_deltanet_hyena_shortconv_grouped_moe_trunk'


def rmsnorm(x, gain):
    x32 = x.astype(jnp.float32)
    y = x32 * lax.rsqrt(jnp.mean(x32 * x32, axis=-1, keepdims=True) + EPS)
    return (y * gain.astype(jnp.float32)).astype(x.dtype)


def modulate(x, gain, shift, scale):
    return rmsnorm(x, gain) * (1 + scale) + shift


def l2norm(t):
    return t * lax.rsqrt(jnp.sum(t * t, axis=-1, keepdims=True) + EPS)


def short_conv(x, w, on_grid):
    b, l, ch = x.shape
    xs = x.reshape(b, l // GRID_W, GRID_W, ch) if on_grid else x.reshape(b, 1, l, ch)
    n = xs.shape[2]
    xp = jnp.pad(xs, ((0, 0), (0, 0), (1, 1), (0, 0)))
    y = w[0] * xp[:, :, 0:n] + w[1] * xp[:, :, 1:n + 1] + w[2] * xp[:, :, 2:n + 2]
    return y.reshape(b, l, ch)


def gated_delta_chunked(q, k, v, g, beta, s0):
    b, h, l, dk = q.shape
    dv = v.shape[-1]
    c = DN_CHUNK
    n = l // c
    q = q.reshape(b, h, n, c, dk)
    k = k.reshape(b, h, n, c, dk)
    v = v.reshape(b, h, n, c, dv)
    g = jnp.cumsum(g.reshape(b, h, n, c), axis=-1)
    beta = beta.reshape(b, h, n, c, 1)
    pos = jnp.arange(c)
    incl = pos[:, None] >= pos[None, :]
    strict = pos[:, None] > pos[None, :]
    decay = jnp.exp(jnp.where(incl, g[..., :, None] - g[..., None, :], -jnp.inf))
    kb = k * beta
    a_mat = jnp.einsum('bhnid,bhnjd->bhnij', kb, k) * jnp.where(strict, decay, 0.0)
    rhs = jnp.concatenate([v * beta, kb * jnp.exp(g)[..., None]], axis=-1)
    sol = lax.linalg.triangular_solve(a_mat + jnp.eye(c, dtype=a_mat.dtype), rhs,
                                      left_side=True, lower=True, unit_diagonal=True)
    u, w = sol[..., :dv], sol[..., dv:]
    attn = jnp.einsum('bhnid,bhnjd->bhnij', q, k) * decay
    g_last = g[..., -1:]
    q_dec = q * jnp.exp(g)[..., None]
    k_dec = k * jnp.exp(g_last - g)[..., None]

    def step(s, inp):
        qd, kd, uu, ww, at, gl = inp
        v_new = uu - jnp.einsum('bhck,bhkv->bhcv', ww, s)
        o = jnp.einsum('bhck,bhkv->bhcv', qd, s) + jnp.einsum('bhcs,bhsv->bhcv', at, v_new)
        s = s * jnp.exp(gl)[..., None] + jnp.einsum('bhck,bhcv->bhkv', kd, v_new)
        return s, o

    xs = tuple(jnp.moveaxis(t, 2, 0) for t in (q_dec, k_dec, u, w, attn, g_last))
    s_final, o = lax.scan(step, s0, xs)
    o = jnp.moveaxis(o, 0, 2).reshape(b, h, l, dv)
    return o, s_final


def deltanet_mixer(h_ctx, h_lat, w_in, conv_w, a_log, dt_bias, out_norm, w_out, need_ctx_out):
    d = D_MODEL
    nh = DN_HEADS
    f32 = jnp.float32

    def project(h, on_grid):
        b, l, _ = h.shape
        p = h @ w_in
        qkv = jax.nn.silu(short_conv(p[..., :3 * d], conv_w, on_grid))
        z = p[..., 3 * d:4 * d]
        a = p[..., 4 * d:4 * d + 2 * nh].reshape(b, l, 2, nh).astype(f32)
        bb = p[..., 4 * d + 2 * nh:].reshape(b, l, 2, nh).astype(f32)

        def heads(t):
            return jnp.transpose(t.reshape(b, l, nh, -1), (0, 2, 1, 3)).astype(f32)

        q, k, v = (heads(t) for t in jnp.split(qkv, 3, axis=-1))
        q = l2norm(q) * DN_DK ** -0.5
        k = l2norm(k)
        g = -jnp.exp(a_log.astype(f32)) * jax.nn.softplus(a + dt_bias.astype(f32))
        g = jnp.transpose(g, (2, 0, 3, 1))
        beta = jnp.transpose(jax.nn.sigmoid(bb), (2, 0, 3, 1))
        return q, k, v, g, beta, z

    def scan_both(q, k, v, g, beta, s_f, s_b):
        o_f, s_f = gated_delta_chunked(q, k, v, g[0], beta[0], s_f)
        rev = lambda t: jnp.flip(t, axis=2)
        o_b, s_b = gated_delta_chunked(rev(q), rev(k), rev(v), rev(g[1]), rev(beta[1]), s_b)
        return o_f + rev(o_b), s_f, s_b

    def finish(o, z, dtype):
        b, _, l, _ = o.shape
        o = jnp.transpose(o, (0, 2, 1, 3))
        o = o * lax.rsqrt(jnp.mean(o * o, axis=-1, keepdims=True) + EPS) * out_norm.astype(f32)
        o = o * jax.nn.silu(z.reshape(b, l, nh, DN_DV).astype(f32))
        return o.reshape(b, l, d).astype(dtype) @ w_out

    qc, kc, vc, gc, bc, zc = project(h_ctx, False)
    s0 = jnp.zeros((h_ctx.shape[0], nh, DN_DK, DN_DV), f32)
    o_c, s_f, s_b = scan_both(qc, kc, vc, gc, bc, s0, s0)
    ql, kl, vl, gla, bl, zl = project(h_lat, True)
    o_l, _, _ = scan_both(ql, kl, vl, gla, bl, s_f, s_b)
    y_lat = finish(o_l, zl, h_lat.dtype)
    y_ctx = finish(o_c, zc, h_ctx.dtype) if need_ctx_out else None
    return y_ctx, y_lat


def hyena_filters(l, w1, b1, freq, w2, b2, w3):
    f32 = jnp.float32
    pos = jnp.arange(l, dtype=f32)[:, None]
    t = pos / max(l - 1, 1)
    bands = jnp.linspace(1e-4, HY_BANDS - 1, HY_BANDS, dtype=f32)[None, :]
    ang = (2 * math.pi / l) * pos * bands
    feat = jnp.concatenate([t, jnp.cos(ang), -jnp.sin(ang)], axis=-1)
    fr = freq.astype(f32)
    hdn = jnp.sin(fr * (feat @ w1.astype(f32) + b1.astype(f32)))
    hdn = jnp.sin(fr * (hdn @ w2.astype(f32) + b2.astype(f32)))
    filt = (hdn @ w3.astype(f32)).reshape(l, HY_ORDER, 2, D_MODEL)
    deltas = jnp.abs(jnp.linspace(math.log(HY_TARGET) / HY_SLOW, math.log(HY_TARGET) / HY_FAST,
                                  D_MODEL, dtype=f32))
    window = jnp.exp(-t * deltas[None, :])
    return filt * window[:, None, None, :]


def two_sided_fftconv(u, h_fwd, h_bwd):
    l = u.shape[1]
    k = jnp.concatenate([h_fwd, jnp.zeros_like(h_fwd[:1]), jnp.flip(h_bwd[1:], axis=0)], axis=0)
    kf = jnp.fft.rfft(k, axis=0)
    uf = jnp.fft.rfft(u, n=2 * l, axis=1)
    return jnp.fft.irfft(uf * kf[None], n=2 * l, axis=1)[:, :l]


def hyena_stream(h, on_grid, w_in, conv_w, f_w1, f_b1, f_freq, f_w2, f_b2, f_w3, bias, w_out):
    l = h.shape[1]
    p = short_conv(h @ w_in, conv_w, on_grid)
    v, x1, x2 = jnp.split(p, 3, axis=-1)
    filt = hyena_filters(l, f_w1, f_b1, f_freq, f_w2, f_b2, f_w3)
    z = v.astype(jnp.float32)
    for n, gate in enumerate((x1, x2)):
        conv = two_sided_fftconv(z, filt[:, n, 0], filt[:, n, 1])
        z = gate.astype(jnp.float32) * (conv + bias[n].astype(jnp.float32) * z)
    return z.astype(h.dtype) @ w_out


def shortconv_stream(h, on_grid, w_in, conv_w, w_out):
    bg, cg, xin = jnp.split(h @ w_in, 3, axis=-1)
    return (bg * short_conv(cg * xin, conv_w, on_grid)) @ w_out


def route(h, w_router, router_bias):
    t = h.shape[0]
    scores = jax.nn.sigmoid(h.astype(jnp.float32) @ w_router.astype(jnp.float32))
    choice = (scores + router_bias.astype(jnp.float32)).reshape(t, N_GROUPS, EXPERTS_PER_GROUP)
    group_score = lax.top_k(choice, GROUP_SCORE_K)[0].sum(-1)
    group = jnp.argmax(group_score, axis=-1)
    in_group = jnp.take_along_axis(choice, group[:, None, None], axis=1)[:, 0]
    local = lax.top_k(in_group, TOP_K)[1]
    expert = group[:, None] * EXPERTS_PER_GROUP + local
    weight = jnp.take_along_axis(scores, expert, axis=1)
    weight = weight / jnp.sum(weight, axis=-1, keepdims=True)
    return expert.astype(jnp.int32), weight


def moe_ffn(x, w_router, router_bias, w_gate, w_up, w_down):
    t, d = x.shape
    expert, weight = route(x, w_router, router_bias)
    a = t * TOP_K
    e_flat = expert.reshape(-1)
    order = jnp.argsort(e_flat)
    e_sorted = e_flat[order]
    tok_sorted = (order // TOP_K).astype(jnp.int32)
    w_sorted = weight.reshape(-1)[order]
    counts = jnp.zeros((N_EXPERTS,), jnp.int32).at[e_flat].add(1)
    start = jnp.cumsum(counts) - counts
    padded = (counts + MOE_BLOCK - 1) // MOE_BLOCK * MOE_BLOCK
    pend = jnp.cumsum(padded)
    pstart = pend - padded
    dest = pstart[e_sorted] + (jnp.arange(a, dtype=jnp.int32) - start[e_sorted])
    n_blocks = -(-a // MOE_BLOCK) + N_EXPERTS
    n_slots = n_blocks * MOE_BLOCK
    slot_tok = jnp.full((n_slots,), t, jnp.int32).at[dest].set(tok_sorted)
    slot_w = jnp.zeros((n_slots,), jnp.float32).at[dest].set(w_sorted)
    block_start = jnp.arange(n_blocks, dtype=jnp.int32) * MOE_BLOCK
    block_expert = jnp.minimum(jnp.searchsorted(pend, block_start, side='right'), N_EXPERTS - 1)
    x_pad = jnp.concatenate([x, jnp.zeros((1, d), x.dtype)], axis=0)
    xs = x_pad[slot_tok].reshape(n_blocks, MOE_BLOCK, d)

    def expert_block(args):
        e, xb = args
        hid = jax.nn.silu(xb @ w_gate[e]) * (xb @ w_up[e])
        return hid @ w_down[e]

    ys = lax.map(expert_block, (block_expert, xs)).reshape(n_slots, d)
    ys = ys * slot_w[:, None].astype(ys.dtype)
    return jnp.zeros((t + 1, d), ys.dtype).at[slot_tok].add(ys)[:t]


def setup_inputs(seed: int = 0) -> dict:
    key = jax.random.key(seed)
    ks = iter(jax.random.split(key, 48))
    f32 = jnp.float32

    def nrm(shape, s):
        return jax.random.normal(next(ks), shape, f32) * s

    d = D_MODEL
    inv = d ** -0.5
    dt = jnp.exp(jax.random.uniform(next(ks), (N_A, 2, DN_HEADS), f32, math.log(1e-3), math.log(1e-1)))
    dn_dt_bias = dt + jnp.log(-jnp.expm1(-dt))
    dn_a_log = jnp.log(jax.random.uniform(next(ks), (N_A, 2, DN_HEADS), f32, 1.0, 16.0))
    return {
        'x': nrm((BATCH, SEQ, d), 1.0),
        'c': nrm((BATCH, d), 1.0),
        'ctx': nrm((BATCH, CTX_LEN, d), 1.0),
        'c_ctx': nrm((d,), 1.0),
        'ada_w': nrm((DEPTH, d, N_MOD * d), 0.5 * inv),
        'ada_b': nrm((DEPTH, N_MOD * d), 0.02),
        'norm_mix': 1.0 + nrm((DEPTH, d), 0.05),
        'norm_ffn': 1.0 + nrm((DEPTH, d), 0.05),
        'norm_final': 1.0 + nrm((d,), 0.05),
        'dn_w_in': nrm((N_A, d, DN_IN), inv),
        'dn_conv': nrm((N_A, 3, 3 * d), 3 ** -0.5),
        'dn_a_log': dn_a_log,
        'dn_dt_bias': dn_dt_bias,
        'dn_out_norm': 1.0 + nrm((N_A, DN_DV), 0.05),
        'dn_w_out': nrm((N_A, d, d), inv),
        'hy_w_in': nrm((N_B, d, 3 * d), inv),
        'hy_conv': nrm((N_B, 3, 3 * d), 3 ** -0.5),
        'hy_f_w1': nrm((N_B, HY_EMB, HY_HIDDEN), HY_EMB ** -0.5),
        'hy_f_b1': nrm((N_B, HY_HIDDEN), 0.2),
        'hy_f_freq': 1.0 + nrm((N_B, HY_HIDDEN), 0.1),
        'hy_f_w2': nrm((N_B, HY_HIDDEN, HY_HIDDEN), HY_HIDDEN ** -0.5),
        'hy_f_b2': nrm((N_B, HY_HIDDEN), 0.2),
        'hy_f_w3': nrm((N_B, HY_HIDDEN, HY_ORDER * 2 * d), 0.02 * HY_HIDDEN ** -0.5),
        'hy_bias': nrm((N_B, HY_ORDER, d), 0.5),
        'hy_w_out': nrm((N_B, d, d), inv),
        'sc_w_in': nrm((N_C, d, 3 * d), inv),
        'sc_conv': nrm((N_C, 3, d), 3 ** -0.5),
        'sc_w_out': nrm((N_C, d, d), inv),
        'w_router': nrm((d, N_EXPERTS), inv),
        'router_bias': nrm((N_EXPERTS,), 0.01),
        'moe_w_gate': nrm((DEPTH, N_EXPERTS, d, D_EXPERT), inv),
        'moe_w_up': nrm((DEPTH, N_EXPERTS, d, D_EXPERT), inv),
        'moe_w_down': nrm((DEPTH, N_EXPERTS, D_EXPERT, d), D_EXPERT ** -0.5),
    }


def reference(x, c, ctx, c_ctx, ada_w, ada_b, norm_mix, norm_ffn, norm_final,
              dn_w_in, dn_conv, dn_a_log, dn_dt_bias, dn_out_norm, dn_w_out,
              hy_w_in, hy_conv, hy_f_w1, hy_f_b1, hy_f_freq, hy_f_w2, hy_f_b2, hy_f_w3, hy_bias, hy_w_out,
              sc_w_in, sc_conv, sc_w_out,
              w_router, router_bias, moe_w_gate, moe_w_up, moe_w_down):
    d = D_MODEL
    silu_c = jax.nn.silu(c)
    silu_cc = jax.nn.silu(c_ctx)
    for i in range(DEPTH):
        last = i == DEPTH - 1
        kind, j = i % N_MIXERS, i // N_MIXERS
        ml = jnp.split((silu_c @ ada_w[i] + ada_b[i])[:, None, :], N_MOD, axis=-1)
        mc = jnp.split(silu_cc @ ada_w[i] + ada_b[i], N_MOD, axis=-1)
        h_lat = modulate(x, norm_mix[i], ml[0], ml[1])
        h_ctx = modulate(ctx, norm_mix[i], mc[0], mc[1]) if (kind == 0 or not last) else None
        if kind == 0:
            y_ctx, y_lat = deltanet_mixer(h_ctx, h_lat, dn_w_in[j], dn_conv[j], dn_a_log[j], dn_dt_bias[j],
                                          dn_out_norm[j], dn_w_out[j], not last)
        elif kind == 1:
            hy = (hy_w_in[j], hy_conv[j], hy_f_w1[j], hy_f_b1[j], hy_f_freq[j], hy_f_w2[j], hy_f_b2[j],
                  hy_f_w3[j], hy_bias[j], hy_w_out[j])
            y_lat = hyena_stream(h_lat, True, *hy)
            y_ctx = None if last else hyena_stream(h_ctx, False, *hy)
        else:
            scw = (sc_w_in[j], sc_conv[j], sc_w_out[j])
            y_lat = shortconv_stream(h_lat, True, *scw)
            y_ctx = None if last else shortconv_stream(h_ctx, False, *scw)
        x = x + ml[2] * y_lat
        f_lat = modulate(x, norm_ffn[i], ml[3], ml[4])
        if last:
            out = moe_ffn(f_lat.reshape(-1, d), w_router, router_bias, moe_w_gate[i], moe_w_up[i], moe_w_down[i])
            x = x + ml[5] * out.reshape(x.shape)
        else:
            ctx = ctx + mc[2] * y_ctx
            f_ctx = modulate(ctx, norm_ffn[i], mc[3], mc[4])
            n_ctx = ctx.shape[0] * ctx.shape[1]
            tokens = jnp.concatenate([f_ctx.reshape(-1, d), f_lat.reshape(-1, d)], axis=0)
            out = moe_ffn(tokens, w_router, router_bias, moe_w_gate[i], moe_w_up[i], moe_w_down[i])
            ctx = ctx + mc[5] * out[:n_ctx].reshape(ctx.shape)
            x = x + ml[5] * out[n_ctx:].reshape(x.shape)
    return rmsnorm(x, norm_final)
```

```python
import math
from contextlib import ExitStack

import numpy as np
import concourse.bass as bass
import concourse.mybir as mybir
from concourse.bass_utils import run_bass_kernel_spmd

F32 = mybir.dt.float32
BF16 = mybir.dt.bfloat16
AF = mybir.ActivationFunctionType
ALU = mybir.AluOpType
AX = mybir.AxisListType

NCORES = 8
D = 1024
KC = 8
CTX = 256
SEQ = 16384
LQ = 4096
NT = CTX + LQ
NTA = 4 * NT
EPS = 1e-6
NE = 32
DE = 512
DMA_K = 8
CC_K = 8
G4 = [[0, 1, 2, 3], [4, 5, 6, 7]]
G2 = [[0, 4], [1, 5], [2, 6], [3, 7]]
TILES = [(0, CTX)] + [(CTX + 512 * i, 512) for i in range(LQ // 512)]
PASSES = [[0, 1, 2], [3, 4, 5], [6, 7, 8]]


class Sched:
    COMPUTE = ("scalar", "vector", "gpsimd", "tensor")

    def __init__(self, nc, dma_queues=("sync", "gpsimd")):
        self.nc = nc
        self.ops = []
        self.buf = {}
        self.dma_queues = dma_queues
        self.out_dma_ops = []
        self.last = {}
        self.recent_dma = {q: [] for q in dma_queues}
        self.bar = None
        self.bar_done = set()

    def _st(self, t):
        s = self.buf.get(t)
        if s is None:
            s = [None, {}]
            self.buf[t] = s
        return s

    def barrier(self):
        deps = set(v for k, v in self.last.items() if not str(k).startswith("cc"))
        for q in self.dma_queues:
            deps.update(self.recent_dma[q])
        self.bar = deps
        self.bar_done = set()

    def op(self, eng, fn, reads=(), writes=(), kind="c", final=False):
        deps = set()
        for t in reads:
            s = self._st(t)
            if s[0] is not None:
                deps.add(s[0])
        for t in writes:
            s = self._st(t)
            if s[0] is not None:
                deps.add(s[0])
            deps.update(s[1].values())
        if self.bar is not None and eng not in self.bar_done:
            deps |= self.bar
            self.bar_done.add(eng)
        idx = len(self.ops)
        self.ops.append(dict(eng=eng, fn=fn, deps=deps, kind=kind))
        for t in reads:
            s = self._st(t)
            key = (eng, idx) if kind != "c" else eng
            s[1][key] = idx
        for t in writes:
            self.buf[t] = [idx, {}]
        self.last[eng] = idx
        if kind == "d":
            r = self.recent_dma[eng]
            r.append(idx)
            if len(r) > DMA_K:
                r.pop(0)
        if kind == "cc":
            self.last["cc%d" % idx] = idx
        if final:
            self.out_dma_ops.append(idx)
        return idx

    def dma(self, q, out, in_, reads=(), writes=(), final=False, **kw):
        return self.op(q, lambda e: e.dma_start(out=out, in_=in_, **kw), reads, writes, kind="d", final=final)

    def cc(self, kind, groups, in_ap, out_ap, reads=(), writes=()):
        return self.op("gpsimd", lambda e: e.collective_compute(kind, ALU.bypass, replica_groups=groups, ins=[in_ap], outs=[out_ap]),
                       reads, writes, kind="cc")

    def emit(self, stack):
        nc = self.nc
        ops = self.ops
        needed = set()
        for o in ops:
            needed.update(o["deps"])
        sems = {e: stack.enter_context(nc.semaphore("s_" + e)) for e in self.COMPUTE}
        dsems = {q: [stack.enter_context(nc.semaphore("d_%s_%d" % (q, i))) for i in range(DMA_K)]
                 for q in self.dma_queues}
        cnt = {e: 0 for e in self.COMPUTE}
        dcnt = {q: 0 for q in self.dma_queues}
        ev = {}
        ncc = 0
        ccsems = []
        for i, o in enumerate(ops):
            if o["kind"] == "d":
                q = o["eng"]
                j = dcnt[q]
                dcnt[q] += 1
                o["dslot"] = j
                ev[i] = (dsems[q][j % DMA_K], 16 * (j // DMA_K + 1), ("d", q, j % DMA_K))
            elif o["kind"] == "cc":
                if ncc < CC_K:
                    ccsems.append(stack.enter_context(nc.semaphore("cc%d" % ncc)))
                o["ccslot"] = ncc
                ev[i] = (ccsems[ncc % CC_K], ncc // CC_K + 1, ("cc", ncc % CC_K))
                ncc += 1
            else:
                e = o["eng"]
                if i in needed:
                    cnt[e] += 1
                    ev[i] = (sems[e], cnt[e], ("c", e))
                    o["sig"] = True
                else:
                    o["sig"] = False
        per_eng = {}
        for i, o in enumerate(ops):
            per_eng.setdefault(o["eng"], []).append(i)
        final_waits = [ev[i] for i in self.out_dma_ops]
        if final_waits and "sync" not in per_eng:
            per_eng["sync"] = []

        def replay(engname):
            def body(e):
                seen = {}
                for i in per_eng.get(engname, []):
                    o = ops[i]
                    waits = []
                    for d in o["deps"]:
                        od = ops[d]
                        if od["eng"] == engname and engname == "tensor" and od["kind"] == "c":
                            continue
                        waits.append(ev[d])
                    if o["kind"] == "d":
                        j = o["dslot"]
                        if j >= DMA_K:
                            waits.append((dsems[engname][j % DMA_K], 16 * (j // DMA_K), ("d", engname, j % DMA_K)))
                    if o["kind"] == "cc":
                        j = o["ccslot"]
                        if j >= CC_K:
                            waits.append((ccsems[j % CC_K], j // CC_K, ("cc", j % CC_K)))
                    best = {}
                    for (s, v, k) in waits:
                        if seen.get(k, 0) >= v:
                            continue
                        if k not in best or best[k][1] < v:
                            best[k] = (s, v)
                    for k, (s, v) in best.items():
                        e.wait_ge(s, v)
                        seen[k] = v
                    ins = o["fn"](e)
                    if o["kind"] == "d":
                        ins.then_inc(ev[i][0], 16)
                    elif o["kind"] == "cc":
                        ins.then_inc(ev[i][0])
                    elif o["sig"]:
                        ins.then_inc(ev[i][0], 1)
                if engname == "sync":
                    for (s, v, k) in final_waits:
                        if seen.get(k, 0) < v:
                            e.wait_ge(s, v)
                            seen[k] = v
            return body

        with nc.Block() as block:
            for engname in per_eng:
                getattr(block, engname)(replay(engname))
        return dict(n_ops=len(ops), per_eng={k: len(v) for k, v in per_eng.items()})


class T:
    def __init__(self, ap, name):
        self.ap = ap
        self.name = name

    def __getitem__(self, k):
        return self.ap[k]


def _dsize(dt):
    return 2 if dt == BF16 else 4


class Ctx:
    ARENA = 196 * 1024

    def __init__(self):
        self.nc = bass.Bass("TRN2", target_bir_lowering=False)
        self.st = ExitStack()
        self.S = Sched(self.nc)
        self.qi = 0
        self.arena = self.st.enter_context(self.nc.sbuf_tensor("arena", [128, self.ARENA // 4], F32))
        self.off = 0
        self.gen = 0
        self.PS = [T(self.st.enter_context(self.nc.psum_tensor("P%d" % i, [128, 512], F32)), "P%d" % i) for i in range(8)]

    def din(self, name, shape, dt=F32):
        return self.nc.dram_tensor(name, list(shape), dt, kind="ExternalInput").ap()

    def dout(self, name, shape, dt=F32):
        return self.nc.dram_tensor(name, list(shape), dt, kind="ExternalOutput").ap()

    def dscr(self, name, shape, dt=F32):
        return self.nc.dram_tensor(name, list(shape), dt, kind="Internal").ap()

    def mark(self):
        return self.off

    def release(self, mark):
        self.off = mark
        self.gen += 1
        self.S.barrier()

    def sb(self, name, shape, dt=F32):
        nel = 1
        for s in shape[1:]:
            nel *= s
        nbytes = (nel * _dsize(dt) + 31) // 32 * 32
        a = self.off
        self.off += nbytes
        assert self.off <= self.ARENA, "SBUF arena overflow at %s: %d" % (name, self.off)
        ap = self.arena[0:shape[0], a // 4:(a + nbytes) // 4]
        if dt != F32:
            ap = ap.bitcast(dt)
        ap = ap[:, 0:nel]
        if len(shape) == 3:
            ap = ap.rearrange("p (a b) -> p a b", a=shape[1])
        elif len(shape) == 4:
            ap = ap.rearrange("p (a b c) -> p a b c", a=shape[1], b=shape[2])
        return T(ap, "%s@%d" % (name, self.gen))

    def q(self):
        self.qi += 1
        return ("sync", "gpsimd")[self.qi % 2]

    def finish(self):
        info = self.S.emit(self.st)
        self.st.close()
        return self.nc, info


def fm(ap):
    return ap.rearrange("(c p) t -> p c t", p=128)


def emit_norm_mod(C, xt, w, A, B, ones_bf, sq, ssP, rstd, out_bf=None, out_off=0, out_eng="gpsimd", in_place=True, dst=None):
    S = C.S
    xin = [xt.name + "c%d" % c for c in range(KC)]
    S.op("scalar", lambda e: e.activation(out=sq[:, :, :w], in_=xt[:, :, :w], func=AF.Square), reads=xin, writes=[sq.name])
    for c in range(KC):
        S.op("tensor", lambda e, c=c: e.matmul(ssP[:, :w], lhsT=ones_bf[:], rhs=sq[:, c, :w], start=(c == 0), stop=(c == KC - 1)),
             reads=[sq.name, ones_bf.name], writes=[ssP.name])
    S.op("scalar", lambda e: e.activation(out=rstd[:, :w], in_=ssP[:, :w], func=AF.Sqrt, scale=1.0 / D, bias=C.epsc[:, 0:1]),
         reads=[ssP.name], writes=[rstd.name])
    S.op("vector", lambda e: e.reciprocal(out=rstd[:, :w], in_=rstd[:, :w]),
         reads=[rstd.name], writes=[rstd.name])
    d = xt if dst is None else dst
    for c in range(KC):
        S.op("vector", lambda e, c=c: e.tensor_tensor(out=d[:, c, :w], in0=xt[:, c, :w], in1=rstd[:, :w], op=ALU.mult),
             reads=[xt.name + "c%d" % c, rstd.name], writes=[d.name + "n%d" % c])
        S.op("scalar", lambda e, c=c: e.activation(out=d[:, c, :w], in_=d[:, c, :w], func=AF.Identity, scale=A[:, c:c + 1], bias=B[:, c:c + 1]),
             reads=[d.name + "n%d" % c, "AB"], writes=[d.name + "f%d" % c])
        if out_bf is not None:
            S.op(out_eng, lambda e, c=c: e.tensor_copy(out=out_bf[:, c, out_off:out_off + w], in_=d[:, c, :w]),
                 reads=[d.name + "f%d" % c], writes=[out_bf.name + "c%d" % c])


def load_cast_weight(C, dst, dst_tok, src_ap, rows, cols, stg_list, si, eng="gpsimd"):
    S = C.S
    nch = rows // 128
    per = max(1, 2048 // cols)
    c0 = 0
    while c0 < nch:
        n = min(per, nch - c0)
        sg = stg_list[si[0] % len(stg_list)]
        si[0] += 1
        S.dma(C.q(), sg[:, :n * cols].rearrange("p (c n) -> p c n", c=n), src_ap[c0 * 128:(c0 + n) * 128, :].rearrange("(c p) n -> p c n", p=128), writes=[sg.name])
        S.op(eng, lambda e, sg=sg, c0=c0, n=n: e.tensor_copy(out=dst[:, c0:c0 + n, :], in_=sg[:, :n * cols].rearrange("p (c n) -> p c n", c=n)),
             reads=[sg.name], writes=[dst_tok])
        c0 += n


def emit_router(C, xt, w, wr, rb, P, gate_all, sub0, tmp):
    S = C.S
    nsub = w // 128
    sg, ch, psum, pmin, gs, m2, gmax, og, sel, wsum = tmp
    V = lambda t: t[:].rearrange("p (g k) -> p g k", k=4)
    for s in range(nsub):
        for c in range(KC):
            S.op("tensor", lambda e, s=s, c=c: e.matmul(P[:, :NE], lhsT=xt[:, c, s * 128:(s + 1) * 128], rhs=wr[:, c, :], start=(c == 0), stop=(c == KC - 1)),
                 reads=[xt.name + "f%d" % c, wr.name], writes=[P.name])
        g = gate_all[:, sub0 + s, :]
        S.op("scalar", lambda e: e.activation(out=sg[:], in_=P[:, :NE], func=AF.Sigmoid), reads=[P.name], writes=["sg"])
        S.op("vector", lambda e: e.tensor_tensor(out=ch[:], in0=sg[:], in1=rb[:], op=ALU.add), reads=["sg", rb.name], writes=["ch"])
        for (op, dst, nm) in ((ALU.add, psum, "psum"), (ALU.min, pmin, "pmin")):
            S.op("vector", lambda e, op=op, dst=dst: e.tensor_tensor(out=dst[:, :, 0:2], in0=V(ch)[:, :, 0:4:2], in1=V(ch)[:, :, 1:4:2], op=op), reads=["ch"], writes=[nm + "a"])
            S.op("vector", lambda e, op=op, dst=dst: e.tensor_tensor(out=dst[:, :, 2:4], in0=V(ch)[:, :, 0:2], in1=V(ch)[:, :, 2:4], op=op), reads=["ch"], writes=[nm + "b"])
            S.op("vector", lambda e, op=op, dst=dst: e.tensor_tensor(out=dst[:, :, 4:5], in0=V(ch)[:, :, 0:1], in1=V(ch)[:, :, 3:4], op=op), reads=["ch"], writes=[nm + "c"])
            S.op("vector", lambda e, op=op, dst=dst: e.tensor_tensor(out=dst[:, :, 5:6], in0=V(ch)[:, :, 1:2], in1=V(ch)[:, :, 2:3], op=op), reads=["ch"], writes=[nm + "d"])
        S.op("vector", lambda e: e.tensor_reduce(out=gs[:], in_=psum[:], axis=AX.X, op=ALU.max), reads=["psuma", "psumb", "psumc", "psumd"], writes=["gs"])
        S.op("vector", lambda e: e.tensor_reduce(out=m2[:], in_=pmin[:], axis=AX.X, op=ALU.max), reads=["pmina", "pminb", "pminc", "pmind"], writes=["m2"])
        S.op("vector", lambda e: e.tensor_reduce(out=gmax[:], in_=gs[:], axis=AX.X, op=ALU.max), reads=["gs"], writes=["gmax"])
        S.op("vector", lambda e: e.tensor_scalar(out=og[:], in0=gs[:], scalar1=gmax[:, 0:1], scalar2=None, op0=ALU.is_ge), reads=["gs", "gmax"], writes=["og"])
        for k in range(4):
            S.op("vector", lambda e, k=k: e.tensor_tensor(out=sel[:, :, k], in0=V(ch)[:, :, k], in1=m2[:], op=ALU.is_ge), reads=["ch", "m2"], writes=["sel%d" % k])
            S.op("vector", lambda e, k=k: e.tensor_tensor(out=sel[:, :, k], in0=sel[:, :, k], in1=og[:], op=ALU.mult), reads=["sel%d" % k, "og"], writes=["selm%d" % k])
        selm = ["selm%d" % k for k in range(4)]
        S.op("vector", lambda e: e.tensor_tensor(out=sg[:], in0=sg[:], in1=sel[:].rearrange("p g k -> p (g k)"), op=ALU.mult), reads=["sg"] + selm, writes=["sg"])
        S.op("vector", lambda e: e.tensor_reduce(out=wsum[:], in_=sg[:], axis=AX.X, op=ALU.add), reads=["sg"], writes=["wsum"])
        S.op("vector", lambda e: e.reciprocal(out=wsum[:], in_=wsum[:]), reads=["wsum"], writes=["wsum"])
        S.op("vector", lambda e, g=g: e.tensor_scalar(out=g, in0=sg[:], scalar1=wsum[:, 0:1], scalar2=None, op0=ALU.mult), reads=["sg", "wsum"], writes=[gate_all.name])


class Prog:
    def __init__(self, layers=(0, 1, 2, 3), final_norm=True, do_moe=True, do_mixer=True):
        self.layers = list(layers)
        self.C = C = Ctx()
        self.S = C.S
        L = self.layers
        self.xT = C.din("xT", [D, NT])
        self.cT = C.din("cT", [128, 3])
        self.ada_w = C.din("ada_w", [4, 128, 6 * D])
        self.ada_bT = C.din("ada_bT", [128, 192])
        self.bsel = C.din("bsel", [128, 2])
        self.rsel = C.din("rsel", [128, 4])
        self.gains = C.din("gains", [128, 9, KC])
        self.w_router = C.din("w_router", [D, NE])
        self.rbias = C.din("rbias", [NE])
        self.ident_in = C.din("ident", [128, 128])
        self.moe_w = {}
        if do_moe:
            for l in L:
                self.moe_w[l] = (C.din("wg%d" % l, [4, D, DE]), C.din("wu%d" % l, [4, D, DE]), C.din("wd%d" % l, [4, DE, D]))
        self.mix_w = {}
        if do_mixer:
            for l in L:
                if l == 2:
                    self.mix_w[l] = dict(w_in=C.din("sc_w_in", [D, 3 * D]), conv=C.din("sc_convT", [128, KC, 3]), w_out=C.din("w_out2", [D, D]))
                if l == 1:
                    self.mix_w[l] = dict(
                        w_in=C.din("hy_w_in", [D, 768]), conv=C.din("hy_convT", [128, 6, 3]), w_out=C.din("w_out1", [D, D]),
                        f_w1=C.din("hy_f_w1", [33, 64]), f_b1=C.din("hy_f_b1", [64, 1]), f_fr=C.din("hy_f_fr", [64, 1]),
                        f_w2=C.din("hy_f_w2", [64, 64]), f_b2=C.din("hy_f_b2", [64, 1]), f_w3=C.din("hy_f_w3", [64, 8, 128]),
                        bias=C.din("hy_biasT", [128, 2, 2]), ndelta=C.din("hy_ndelta", [128, 2]),
                        feat_lat=C.din("hy_feat_lat", [33, 2 * SEQ]), feat_ctx=C.din("hy_feat_ctx", [33, 2 * CTX]))
            for l in L:
                if l in (0, 3):
                    self.mix_w[l] = dict(w_qkvz=C.din("dn_w_qkvz%d" % l, [D, 1024]), w_ab=C.din("dn_w_ab%d" % l, [D, 8]), conv=C.din("dn_convT%d" % l, [128, 6, 3]),
                                         alog_bc=C.din("dn_alog%d" % l, [128, 4]), dtb_bc=C.din("dn_dtb%d" % l, [128, 4]), masks=C.din("dn_masks%d" % l, [128, 8, 128]), bmasks=C.din("dn_bmasks%d" % l, [128, 5, 128]),
                                         onorm_bc=C.din("dn_onorm%d" % l, [128, 128]), w_out=C.din("w_out%d" % l, [D, D]))
            if any(l in (0, 3) for l in L):
                NS = CTX + SEQ
                dbg = False
                mk_ = (lambda n, sh, dt=F32: C.dout(n, sh, dt)) if dbg else (lambda n, sh, dt=F32: C.dscr(n, sh, dt))
                self.dq = [mk_("dn_q%d" % h, [128, NS], BF16) for h in range(2)]
                self.dk = [mk_("dn_k%d" % h, [128, NS], BF16) for h in range(2)]
                self.dkt = [C.dscr("dn_kt%d" % h, [NS, 128], BF16) for h in range(2)]
                self.dvt = [C.dscr("dn_vt%d" % h, [NS, 128], BF16) for h in range(2)]
                self.dz = C.dscr("dn_z", [NS, 256], BF16)
                self.dof = [mk_("dn_of%d" % h, [NS, 128]) for h in range(2)]
                self.dob = [mk_("dn_ob%d" % h, [NS, 128]) for h in range(2)]
            if any(l in (0, 1, 3) for l in L):
                self.anti_in = C.din("anti", [128, 128])
                PW = (2304, 2048)
                self.PW = PW
                self.hs = [[C.dscr("hs_%d_%d" % (c, p), [128, PW[p]], BF16) for p in range(2)] for c in range(KC)]
                self.hall = [[C.dscr("hall_%d_%d" % (c, p), [512, PW[p]], BF16) for p in range(2)] for c in range(KC)]
                self.os = [[[C.dscr("os_%d_%d_%d" % (cc, j, p), [128, PW[p]], BF16) for p in range(2)] for j in range(4)] for cc in range(2)]
                self.oall = [[[C.dscr("oall_%d_%d_%d" % (cc, j, p), [512, PW[p]], BF16) for p in range(2)] for j in range(4)] for cc in range(2)]
                if 1 in L:
                    self.K2h = [[self.C.nc.dram_tensor("K2_%d_%d" % (n, cc), [128, 2 * SEQ], BF16, kind="Internal") for cc in range(2)] for n in range(2)]
                    self.K2ch = [[self.C.nc.dram_tensor("K2c_%d_%d" % (n, cc), [128, 2 * CTX], BF16, kind="Internal") for cc in range(2)] for n in range(2)]
        self.xo = C.dout("xo", [D, NT])
        self.xbuf = C.dscr("xbuf", [D, NT])
        self.wsh = {l: [C.dscr("wsh%d_%d" % (l, p), [128, 4096], BF16) for p in range(12)] for l in self.moe_w}
        self.wg4 = {l: [C.dscr("wg4_%d_%d" % (l, p), [512, 4096], BF16) for p in range(12)] for l in self.moe_w}
        self.wfull = {l: [[C.dscr("wf%d_%d_%d" % (l, p, h), [512, 4096], BF16) for h in range(2)] for p in range(12)] for l in self.moe_w}
        self.modp = C.dscr("modp", [128 * 3, 192])
        self.modg4 = C.dscr("modg4", [4 * 128 * 3, 192])
        self.modg8 = C.dscr("modg8", [8 * 128 * 3, 192])
        self.do_moe = do_moe
        self.do_mixer = do_mixer
        self.final_norm = final_norm
        self.build()

    def build(self):
        C, S = self.C, self.S
        self.ident = C.sb("ident", [128, 128])
        self.ones_bf = C.sb("ones_bf", [128, 128], BF16)
        self.ones_f = C.sb("ones_f", [128, 128])
        self.mod = C.sb("mod", [128, 192, 2])
        self.gain = C.sb("gain", [128, 9, KC])
        self.AB = C.sb("AB", [128, 2, 2, KC])
        self.wr = C.sb("wr", [128, KC, NE])
        self.rb = C.sb("rb", [128, NE])
        self.rselt = C.sb("rselt", [128, 4])
        S.dma("sync", self.ident[:], self.ident_in, writes=[self.ident.name])
        S.dma("sync", self.gain[:], self.gains, writes=[self.gain.name])
        S.dma("sync", self.wr[:], self.w_router.rearrange("(c p) n -> p c n", p=128), writes=[self.wr.name])
        S.dma("sync", self.rb[:], self.rbias.partition_broadcast(128), writes=[self.rb.name])
        S.dma("sync", self.rselt[:], self.rsel, writes=[self.rselt.name])
        self.ident_bf = C.sb("ident_bf", [128, 128], BF16)
        S.op("vector", lambda e: e.tensor_copy(out=self.ident_bf[:], in_=self.ident[:]), reads=[self.ident.name], writes=[self.ident_bf.name])
        if hasattr(self, "anti_in"):
            self.anti_f = C.sb("anti_f", [128, 128])
            self.anti_bf = C.sb("anti_bf", [128, 128], BF16)
            S.dma("sync", self.anti_f[:], self.anti_in, writes=[self.anti_f.name])
            S.op("vector", lambda e: e.tensor_copy(out=self.anti_bf[:], in_=self.anti_f[:]), reads=[self.anti_f.name], writes=[self.anti_bf.name])
        S.op("gpsimd", lambda e: e.memset(self.ones_bf[:], 1.0), writes=[self.ones_bf.name])
        S.op("gpsimd", lambda e: e.memset(self.ones_f[:], 1.0), writes=[self.ones_f.name])
        C.epsc = C.sb("epsc", [128, 1])
        S.op("gpsimd", lambda e: e.memset(C.epsc[:], EPS), writes=[C.epsc.name])
        for c in range(KC):
            S.dma(C.q(), self.xbuf[c * 128:(c + 1) * 128, :], self.xT[c * 128:(c + 1) * 128, :], writes=["xbuf%d" % i for i in range(len(TILES))] if c == KC - 1 else ["xbufpart%d" % c])
        base_mark = C.mark()
        self.phase_mod()
        C.release(base_mark)
        if self.do_moe:
            for l in self.layers:
                self.phase_moe_wprep(l)
                C.release(base_mark)
        for l in self.layers:
            if self.do_mixer:
                if l == 2:
                    self.phase_shortconv(l)
                    C.release(base_mark)
                else:
                    self.phase_A(l)
                    C.release(base_mark)
                    if l == 1:
                        self.phase_B_hyena(l, base_mark)
                    else:
                        self.phase_B_deltanet(l, base_mark)
                    C.release(base_mark)
                    self.phase_C(l)
                    C.release(base_mark)
            if self.do_moe:
                self.phase_moe(l)
                C.release(base_mark)
        self.phase_out()

    def phase_mod(self):
        C, S = self.C, self.S
        PS = C.PS
        cs = C.sb("cs", [128, 3])
        sc = C.sb("scl", [128, 3])
        bT = C.sb("bT", [128, 192])
        bs = C.sb("bs", [128, 2])
        part = C.sb("part", [128, 3, 192])
        wst = [C.sb("wst%d" % i, [128, 3072]) for i in range(2)]
        S.dma("sync", cs[:], self.cT, writes=[cs.name])
        S.dma("sync", bT[:], self.ada_bT, writes=[bT.name])
        S.dma("sync", bs[:], self.bsel, writes=[bs.name])
        S.op("scalar", lambda e: e.activation(out=sc[:], in_=cs[:], func=AF.Silu), reads=[cs.name], writes=[sc.name])
        k = 0
        for l in range(4):
            for hb in range(2):
                w = wst[k % 2]
                S.dma(C.q(), w[:], self.ada_w[l, :, hb * 3072:(hb + 1) * 3072], writes=[w.name])
                for j in range(24):
                    P = PS[(k * 24 + j) % 4]
                    col = l * 48 + hb * 24 + j
                    S.op("tensor", lambda e, w=w, P=P, j=j: e.matmul(P[:, 0:3], lhsT=w[:, j * 128:(j + 1) * 128], rhs=sc[:], start=True, stop=True),
                         reads=[w.name, sc.name], writes=[P.name])
                    S.op("vector", lambda e, P=P, col=col: e.tensor_copy(out=part[:, :, col], in_=P[:, 0:3]), reads=[P.name], writes=[part.name])
                k += 1
        for v in range(3):
            S.dma("sync", self.modp[v * 128:(v + 1) * 128, :], part[:, v, :], reads=[part.name], writes=["modp"])
        S.cc("AllGather", G4, self.modp, self.modg4, reads=["modp"], writes=["modg4"])
        S.cc("AllGather", G2, self.modg4, self.modg8, reads=["modg4"], writes=["modg8"])
        allp = C.sb("allp", [128, 8, 3, 192])
        S.dma("sync", allp[:], self.modg8.rearrange("(r v p) c -> p r v c", r=8, v=3), reads=["modg8"], writes=[allp.name])
        tot = C.sb("tot", [128, 3, 192])
        S.op("vector", lambda e: e.tensor_tensor(out=tot[:], in0=allp[:, 0], in1=allp[:, 1], op=ALU.add), reads=[allp.name], writes=[tot.name])
        for r in range(2, 8):
            S.op("vector", lambda e, r=r: e.tensor_tensor(out=tot[:], in0=tot[:], in1=allp[:, r], op=ALU.add), reads=[allp.name, tot.name], writes=[tot.name])
        S.op("vector", lambda e: e.tensor_scalar(out=tot[:, 0, :], in0=tot[:, 0, :], scalar1=bs[:, 0:1], scalar2=None, op0=ALU.mult), reads=[tot.name, bs.name], writes=[tot.name])
        S.op("vector", lambda e: e.scalar_tensor_tensor(out=tot[:, 0, :], in0=tot[:, 1, :], scalar=bs[:, 1:2], in1=tot[:, 0, :], op0=ALU.mult, op1=ALU.add),
             reads=[tot.name, bs.name], writes=[tot.name])
        S.op("vector", lambda e: e.tensor_tensor(out=self.mod[:, :, 0], in0=tot[:, 0, :], in1=bT[:], op=ALU.add), reads=[tot.name, bT.name], writes=[self.mod.name])
        S.op("vector", lambda e: e.tensor_tensor(out=self.mod[:, :, 1], in0=tot[:, 2, :], in1=bT[:], op=ALU.add), reads=[tot.name, bT.name, self.mod.name], writes=[self.mod.name])

    def phase_moe_wprep(self, l):
        C, S = self.C, self.S
        wg, wu, wd = self.moe_w[l]
        stg = [C.sb("pstg%d" % i, [128, 4096]) for i in range(3)]
        cvt = [C.sb("pcvt%d" % i, [128, 4096], BF16) for i in range(3)]
        k = 0
        wtoks = []
        for ex in range(4):
            for m, src in enumerate((wg, wu, wd)):
                sg, cv = stg[k % 3], cvt[k % 3]
                k += 1
                if m < 2:
                    srcv = src[ex].rearrange("(c p) n -> p c n", p=128)
                    S.dma(C.q(), sg[:].rearrange("p (c n) -> p c n", c=8), srcv, writes=[sg.name])
                else:
                    srcv = src[ex].rearrange("(c p) n -> p c n", p=128)
                    S.dma(C.q(), sg[:].rearrange("p (c n) -> p c n", c=4), srcv, writes=[sg.name])
                eng = ("vector", "gpsimd", "scalar")[k % 3]
                if eng == "scalar":
                    S.op(eng, lambda e, sg=sg, cv=cv: e.activation(out=cv[:], in_=sg[:], func=AF.Copy), reads=[sg.name], writes=[cv.name])
                else:
                    S.op(eng, lambda e, sg=sg, cv=cv: e.tensor_copy(out=cv[:], in_=sg[:]), reads=[sg.name], writes=[cv.name])
                p = ex * 3 + m
                S.dma(C.q(), self.wsh[l][p], cv[:], reads=[cv.name], writes=["wsh%d_%d" % (l, p)])
        for p in range(12):
            S.cc("AllGather", G4, self.wsh[l][p], self.wg4[l][p], reads=["wsh%d_%d" % (l, p)], writes=["wg4_%d_%d" % (l, p)])
        for p in range(12):
            for h in range(2):
                S.cc("AllGather", G2, self.wg4[l][p][h * 256:(h + 1) * 256, :], self.wfull[l][p][h], reads=["wg4_%d_%d" % (l, p)], writes=["wf%d_%d_%d" % (l, p, h)])

    def set_AB(self, gain_idx, scale_col, shift_col):
        S = self.S
        for which in range(2):
            S.op("vector", lambda e, which=which: e.scalar_tensor_tensor(out=self.AB[:, which, 0, :], in0=self.mod[:, scale_col:scale_col + 8, which], scalar=1.0,
                                                                         in1=self.gain[:, gain_idx, :], op0=ALU.add, op1=ALU.mult),
                 reads=[self.mod.name, self.gain.name], writes=["AB"])
            S.op("vector", lambda e, which=which: e.tensor_copy(out=self.AB[:, which, 1, :], in_=self.mod[:, shift_col:shift_col + 8, which]),
                 reads=[self.mod.name, "AB"], writes=["AB"])

    def load_x(self, xt, ti):
        t0, w = TILES[ti]
        self.S.dma("sync", xt[:, :, :w], fm(self.xbuf)[:, :, t0:t0 + w], reads=["xbuf%d" % ti] + ["xbufpart%d" % c for c in range(KC - 1)],
                   writes=[xt.name + s + "%d" % c for c in range(KC) for s in ("c", "n", "f")])

    def store_x(self, xt, ti, suffix="f"):
        t0, w = TILES[ti]
        self.S.dma("gpsimd", fm(self.xbuf)[:, :, t0:t0 + w], xt[:, :, :w], reads=[xt.name + suffix + "%d" % c for c in range(KC)], writes=["xbuf%d" % ti])

    def tile_part(self, ti):
        t0, w = TILES[ti]
        p = 0 if ti <= 4 else 1
        return p, t0 - (0 if p == 0 else 2304)

    def phase_A(self, l):
        C, S = self.C, self.S
        PS = C.PS
        base = l * 48
        self.set_AB(l, base + 8, base + 0)
        xt = C.sb("ax", [128, KC, 512])
        sq = C.sb("asq", [128, KC, 512], BF16)
        rstd = C.sb("ar", [128, 512])
        h = [C.sb("ah%d" % i, [128, KC, 512], BF16) for i in range(2)]
        for ti in range(len(TILES)):
            t0, w = TILES[ti]
            which = 1 if ti == 0 else 0
            hh = h[ti % 2]
            self.load_x(xt, ti)
            emit_norm_mod(C, xt, w, self.AB[:, which, 0, :], self.AB[:, which, 1, :], self.ones_bf, sq, PS[0], rstd, dst=hh)
            p, co = self.tile_part(ti)
            for c in range(KC):
                S.dma(C.q(), self.hs[c][p][:, co:co + w], hh[:, c, :w], reads=[hh.name + "f%d" % c], writes=["hs_%d_%d_t%d" % (c, p, ti)])
        for c in range(KC):
            for p in range(2):
                toks = ["hs_%d_%d_t%d" % (c, p, ti) for ti in range(9) if self.tile_part(ti)[0] == p]
                S.cc("AllGather", G4, self.hs[c][p], self.hall[c][p], reads=toks, writes=["hall_%d_%d" % (c, p)])

    def phase_C(self, l):
        C, S = self.C, self.S
        PS = C.PS
        base = l * 48
        w_out = C.sb("cwout", [128, KC, D], BF16)
        stg = [C.sb("cstg%d" % i, [128, 2048]) for i in range(2)]
        si = [0]
        load_cast_weight(C, w_out, w_out.name, self.mix_w[l]["w_out"], D, D, stg, si)
        xt = C.sb("cx", [128, KC, 512])
        osel = C.sb("cosel", [128, KC, 512], BF16)
        og = [C.sb("cog%d" % i, [128, 4, 512], BF16) for i in range(3)]
        k = 0
        for ti in range(len(TILES)):
            if l == 3 and ti == 0:
                continue
            t0, w = TILES[ti]
            which = 1 if ti == 0 else 0
            p, co = self.tile_part(ti)
            self.load_x(xt, ti)
            for cc in range(2):
                for j in range(4):
                    g = og[k % 3]
                    k += 1
                    S.dma(C.q(), g[:, :, :w], self.oall[cc][j][p].rearrange("(i q) t -> q i t", q=128)[:, :, co:co + w], reads=["oall_%d_%d_%d" % (cc, j, p)], writes=[g.name])
                    dst = osel[:, cc:KC:2, :w]
                    if j == 0:
                        S.op("vector", lambda e, g=g, dst=dst, w=w, j=j: e.tensor_scalar(out=dst, in0=g[:, :, :w], scalar1=self.rselt[:, j:j + 1], scalar2=None, op0=ALU.mult),
                             reads=[g.name, self.rselt.name], writes=[osel.name + "%d" % cc])
                    else:
                        S.op("vector", lambda e, g=g, dst=dst, w=w, j=j: e.scalar_tensor_tensor(out=dst, in0=g[:, :, :w], scalar=self.rselt[:, j:j + 1], in1=dst, op0=ALU.mult, op1=ALU.add),
                             reads=[g.name, self.rselt.name, osel.name + "%d" % cc], writes=[osel.name + "%d" % cc])
            for dc in range(KC):
                P = PS[dc % 4]
                for fc in range(KC):
                    S.op("tensor", lambda e, P=P, dc=dc, fc=fc, w=w: e.matmul(P[:, :w], lhsT=w_out[:, fc, dc * 128:(dc + 1) * 128], rhs=osel[:, fc, :w], start=(fc == 0), stop=(fc == KC - 1)),
                         reads=[w_out.name, osel.name + "%d" % (fc % 2)], writes=[P.name])
                S.op("vector", lambda e, P=P, dc=dc, w=w, which=which: e.scalar_tensor_tensor(out=xt[:, dc, :w], in0=P[:, :w], scalar=self.mod[:, base + 16 + dc, which:which + 1], in1=xt[:, dc, :w], op0=ALU.mult, op1=ALU.add),
                     reads=[P.name, self.mod.name, xt.name + "c%d" % dc], writes=[xt.name + "f%d" % dc])
            self.store_x(xt, ti)

    def ag_outputs(self):
        S = self.S
        for cc in range(2):
            for j in range(4):
                for p in range(2):
                    toks = ["os_%d_%d_%d_t%d" % (cc, j, p, ti) for ti in range(9) if self.tile_part(ti)[0] == p]
                    S.cc("AllGather", G4, self.os[cc][j][p], self.oall[cc][j][p], reads=toks, writes=["oall_%d_%d_%d" % (cc, j, p)])

    def phase_B_hyena(self, l, base_mark):
        C = self.C
        W = self.mix_w[l]
        self.hy_filters(W)
        C.release(base_mark)
        for cc in range(2):
            self.hy_chunk(l, W, cc)
            C.release(base_mark)
        self.ag_outputs()

    def hy_filters(self, W):
        C, S = self.C, self.S
        PS = C.PS
        w1 = C.sb("fw1", [64, 64])
        w2 = C.sb("fw2", [64, 64])
        w3 = C.sb("fw3", [64, 8, 128])
        b1 = C.sb("fb1", [64, 1])
        b2 = C.sb("fb2", [64, 1])
        fr = C.sb("ffr", [64, 1])
        bias = C.sb("fbias", [128, 2, 2])
        nd = C.sb("fnd", [128, 2])
        S.dma("sync", w1[0:33, :], W["f_w1"], writes=[w1.name])
        S.dma("sync", w2[:], W["f_w2"], writes=[w2.name])
        S.dma("sync", w3[:], W["f_w3"], writes=[w3.name])
        S.dma("sync", b1[:], W["f_b1"], writes=[b1.name])
        S.dma("sync", b2[:], W["f_b2"], writes=[b2.name])
        S.dma("sync", fr[:], W["f_fr"], writes=[fr.name])
        S.dma("sync", bias[:], W["bias"], writes=[bias.name])
        S.dma("sync", nd[:], W["ndelta"], writes=[nd.name])
        ft = [C.sb("fft%d" % i, [64, 512]) for i in range(2)]
        tb = [C.sb("ftb%d" % i, [128, 512]) for i in range(2)]
        arg = C.sb("farg", [64, 512])
        kq = C.sb("fkq", [64, 512])
        h1 = C.sb("fh1", [64, 512])
        h2 = C.sb("fh2", [64, 512])
        win = [C.sb("fwin%d" % i, [128, 512]) for i in range(2)]
        kt = [C.sb("fkt%d" % i, [128, 512], BF16) for i in range(4)]
        MAGIC = 12582912.0
        TWO_PI = 2.0 * math.pi

        def sinlayer(wT, kdim, xin, xtok, b, out, Pn):
            P = PS[Pn]
            S.op("tensor", lambda e: e.matmul(P[0:64, :], lhsT=wT[0:kdim, :], rhs=xin[0:kdim, :], start=True, stop=True), reads=[wT.name, xtok], writes=[P.name])
            S.op("vector", lambda e: e.tensor_scalar(out=arg[:], in0=P[0:64, :], scalar1=b[:, 0:1], scalar2=fr[:, 0:1], op0=ALU.add, op1=ALU.mult), reads=[P.name, b.name, fr.name], writes=[arg.name])
            S.op("vector", lambda e: e.tensor_scalar(out=kq[:], in0=arg[:], scalar1=1.0 / TWO_PI, scalar2=MAGIC, op0=ALU.mult, op1=ALU.add), reads=[arg.name], writes=[kq.name])
            S.op("vector", lambda e: e.tensor_scalar(out=kq[:], in0=kq[:], scalar1=-MAGIC, scalar2=None, op0=ALU.add), reads=[kq.name], writes=[kq.name])
            S.op("vector", lambda e: e.scalar_tensor_tensor(out=arg[:], in0=kq[:], scalar=-TWO_PI, in1=arg[:], op0=ALU.mult, op1=ALU.add), reads=[kq.name, arg.name], writes=[arg.name])
            S.op("scalar", lambda e: e.activation(out=out[:], in_=arg[:], func=AF.Sin), reads=[arg.name], writes=[out.name])

        kti = 0
        it = 0
        for (feat, L2, Kst) in ((W["feat_lat"], 2 * SEQ, self.K2h), (W["feat_ctx"], 2 * CTX, self.K2ch)):
            ntile = L2 // 512
            ctile = (L2 // 2 - 1) // 512
            for yt in range(ntile):
                f_ = ft[it % 2]
                t_ = tb[it % 2]
                it += 1
                S.dma("sync", f_[0:33, :], feat[:, yt * 512:(yt + 1) * 512], writes=[f_.name])
                S.dma("gpsimd", t_[:], feat[0, yt * 512:(yt + 1) * 512].partition_broadcast(128), writes=[t_.name])
                sinlayer(w1, 33, f_, f_.name, b1, h1, 0)
                sinlayer(w2, 64, h1, h1.name, b2, h2, 1)
                for cc in range(2):
                    S.op("scalar", lambda e, cc=cc, t_=t_: e.activation(out=win[cc][:], in_=t_[:], func=AF.Exp, scale=nd[:, cc:cc + 1]), reads=[t_.name, nd.name], writes=[win[cc].name])
                L1 = L2 // 2 - 1
                c0 = L1 - yt * 512
                for n in range(2):
                    sA, sB = (0, 1) if n == 0 else (1, 0)
                    for cc in range(2):
                        k_ = kt[kti % 4]
                        PA = PS[2 + (kti % 2) * 2]
                        PB = PS[3 + (kti % 2) * 2]
                        kti += 1
                        needA = c0 >= 0
                        needB = c0 < 511 or (c0 < 512 and sA == 1)
                        if needA:
                            S.op("tensor", lambda e, PA=PA, n=n, sA=sA, cc=cc: e.matmul(PA[:, :], lhsT=w3[:, (n * 2 + sA) * 2 + cc, :], rhs=h2[:], start=True, stop=True), reads=[w3.name, h2.name], writes=[PA.name])
                        if needB:
                            S.op("tensor", lambda e, PB=PB, n=n, sB=sB, cc=cc: e.matmul(PB[:, :], lhsT=w3[:, (n * 2 + sB) * 2 + cc, :], rhs=h2[:], start=True, stop=True), reads=[w3.name, h2.name], writes=[PB.name])
                        if c0 >= 512:
                            S.op("vector", lambda e, PA=PA, k_=k_, cc=cc: e.tensor_tensor(out=k_[:], in0=PA[:, :], in1=win[cc][:], op=ALU.mult), reads=[PA.name, win[cc].name], writes=[k_.name])
                        elif c0 < 0:
                            S.op("vector", lambda e, PB=PB, k_=k_, cc=cc: e.tensor_tensor(out=k_[:], in0=PB[:, :], in1=win[cc][:], op=ALU.mult), reads=[PB.name, win[cc].name], writes=[k_.name])
                        else:
                            if c0 > 0:
                                S.op("vector", lambda e, PA=PA, k_=k_, cc=cc, c0=c0: e.tensor_tensor(out=k_[:, 0:c0], in0=PA[:, 0:c0], in1=win[cc][:, 0:c0], op=ALU.mult), reads=[PA.name, win[cc].name], writes=[k_.name])
                            if c0 < 511:
                                S.op("vector", lambda e, PB=PB, k_=k_, cc=cc, c0=c0: e.tensor_tensor(out=k_[:, c0 + 1:512], in0=PB[:, c0 + 1:512], in1=win[cc][:, c0 + 1:512], op=ALU.mult), reads=[PB.name, win[cc].name, k_.name], writes=[k_.name])
                            Pf = PA if sA == 0 else PB
                            S.op("vector", lambda e, Pf=Pf, k_=k_, n=n, cc=cc, c0=c0: e.tensor_scalar(out=k_[:, c0:c0 + 1], in0=Pf[:, c0:c0 + 1], scalar1=bias[:, n, cc:cc + 1], scalar2=None, op0=ALU.add),
                                 reads=[Pf.name, bias.name, k_.name], writes=[k_.name])
                        S.dma(C.q(), Kst[n][cc].ap()[:, yt * 512:(yt + 1) * 512], k_[:], reads=[k_.name], writes=["K2_%d_%d_%d" % (L2, n, cc)])

    def hy_chunk(self, l, W, cc):
        C, S = self.C, self.S
        PS = C.PS
        NJ = 130
        Zv = C.sb("Zv", [128, 128, NJ], BF16)
        Zx1 = C.sb("Zx1", [128, 128, NJ], BF16)
        Zx2 = C.sb("Zx2", [128, 128, NJ], BF16)
        Z = (Zv, Zx1, Zx2)
        mk = C.mark()
        win_b = C.sb("hwin", [128, KC, 384], BF16)
        cw = C.sb("hcw", [128, 6, 3])
        stg = [C.sb("hstg%d" % i, [128, 2048]) for i in range(2)]
        hst = [C.sb("hh%d" % i, [128, KC, 512], BF16) for i in range(2)]
        X = [C.sb("hX%d" % i, [128, 512]) for i in range(3)]
        Xb = [C.sb("hXb%d" % i, [128, 512], BF16) for i in range(3)]
        zt = [C.sb("hzt%d" % i, [128, 128], BF16) for i in range(2)]
        si = [0]
        S.dma("sync", cw[:], W["conv"], writes=[cw.name])
        for kind in range(3):
            for c in range(KC):
                sg = stg[si[0] % 2]
                si[0] += 1
                S.dma(C.q(), sg[:, :128], W["w_in"][c * 128:(c + 1) * 128, kind * 256 + cc * 128:kind * 256 + cc * 128 + 128], writes=[sg.name])
                S.op("gpsimd", lambda e, sg=sg, c=c, kind=kind: e.tensor_copy(out=win_b[:, c, kind * 128:(kind + 1) * 128], in_=sg[:, :128]), reads=[sg.name], writes=[win_b.name])
        k = 0
        nt = 0
        for j in range(4):
            for ti in range(len(TILES)):
                if j > 0 and ti == 0:
                    continue
                t0, w = TILES[ti]
                p, co = self.tile_part(ti)
                hh = hst[k % 2]
                for c in range(KC):
                    S.dma(C.q(), hh[:, c, :w], self.hall[c][p][j * 128:(j + 1) * 128, co:co + w], reads=["hall_%d_%d" % (c, p)], writes=[hh.name + "c%d" % c])
                rowlen = w if ti == 0 else 64
                for kind in range(3):
                    P = PS[kind * 2 + (k % 2)]
                    for c in range(KC):
                        S.op("tensor", lambda e, P=P, c=c, kind=kind, hh=hh, w=w: e.matmul(P[:, :w], lhsT=win_b[:, c, kind * 128:(kind + 1) * 128], rhs=hh[:, c, :w], start=(c == 0), stop=(c == KC - 1)),
                             reads=[win_b.name, hh.name + "c%d" % c], writes=[P.name])
                    Xk = X[kind]
                    col = kind * 2 + cc
                    Pv = P[:, :w].rearrange("p (r k) -> p r k", k=rowlen)
                    Xv = Xk[:, :w].rearrange("p (r k) -> p r k", k=rowlen)
                    S.op("vector", lambda e, Pv=Pv, Xv=Xv, col=col: e.tensor_scalar(out=Xv, in0=Pv, scalar1=cw[:, col, 1:2], scalar2=None, op0=ALU.mult), reads=[P.name, cw.name], writes=[Xk.name])
                    S.op("vector", lambda e, Pv=Pv, Xv=Xv, col=col, rowlen=rowlen: e.scalar_tensor_tensor(out=Xv[:, :, 1:rowlen], in0=Pv[:, :, 0:rowlen - 1], scalar=cw[:, col, 0:1], in1=Xv[:, :, 1:rowlen], op0=ALU.mult, op1=ALU.add),
                         reads=[P.name, cw.name, Xk.name], writes=[Xk.name])
                    S.op("vector", lambda e, Pv=Pv, Xv=Xv, col=col, rowlen=rowlen: e.scalar_tensor_tensor(out=Xv[:, :, 0:rowlen - 1], in0=Pv[:, :, 1:rowlen], scalar=cw[:, col, 2:3], in1=Xv[:, :, 0:rowlen - 1], op0=ALU.mult, op1=ALU.add),
                         reads=[P.name, cw.name, Xk.name], writes=[Xk.name])
                    S.op("gpsimd", lambda e, kind=kind, w=w: e.tensor_copy(out=Xb[kind][:, :w], in_=X[kind][:, :w]), reads=[Xk.name], writes=[Xb[kind].name])
                    for blk in range(w // 128):
                        Jz = blk if ti == 0 else 2 + 32 * j + (t0 - CTX) // 128 + blk
                        Pt = PS[6 + (nt % 2)]
                        nt += 1
                        S.op("tensor", lambda e, Pt=Pt, kind=kind, blk=blk: e.matmul(Pt[:, 0:128], lhsT=Xb[kind][:, blk * 128:(blk + 1) * 128], rhs=self.ident_bf[:], start=True, stop=True),
                             reads=[Xb[kind].name, self.ident_bf.name], writes=[Pt.name])
                        if kind != 1:
                            S.op("scalar", lambda e, Pt=Pt, kind=kind, Jz=Jz: e.activation(out=Z[kind][:, :, Jz], in_=Pt[:, 0:128], func=AF.Copy), reads=[Pt.name], writes=[Z[kind].name])
                        else:
                            z_ = zt[nt % 2]
                            S.op("scalar", lambda e, Pt=Pt, z_=z_: e.activation(out=z_[:], in_=Pt[:, 0:128], func=AF.Copy), reads=[Pt.name], writes=[z_.name])
                            Pt2 = PS[6 + (nt % 2)]
                            nt += 1
                            S.op("tensor", lambda e, Pt2=Pt2, z_=z_: e.matmul(Pt2[:, 0:128], lhsT=self.anti_bf[:], rhs=z_[:], start=True, stop=True), reads=[z_.name, self.anti_bf.name], writes=[Pt2.name])
                            S.op("scalar", lambda e, Pt2=Pt2, Jz=Jz: e.activation(out=Zx1[:, :, Jz], in_=Pt2[:, 0:128], func=AF.Copy), reads=[Pt2.name], writes=[Zx1.name])
                k += 1
        C.release(mk)
        NSEG, KSEG = 5, 51
        Gs = [C.sb("G%d" % i, [128, KSEG * 128], BF16) for i in range(NSEG)]
        Gc = C.sb("Gc", [128, 2, 8, 384], BF16)
        z1 = [C.sb("z1_%d" % i, [128, 128], BF16) for i in range(2)]
        z1c = [C.sb("z1c_%d" % i, [128, 2], BF16) for i in range(2)]
        gi = 0
        for ch in range(128):
            if ch % 8 == 0:
                for n in range(2):
                    src = bass.AP(self.K2ch[n][cc], ch * 2 * CTX, [[1, 128], [2 * CTX, 8], [1, 384]])
                    S.dma(C.q(), Gc[:, n, :, :], src, reads=["K2_%d_%d_%d" % (2 * CTX, n, cc)], writes=[Gc.name + "%d" % n])
            z1t = z1[ch % 2]
            z1ct = z1c[ch % 2]
            for n in range(2):
                P = PS[(2 * ch + n) % 4]
                for k in range(255):
                    sidx, kk_ = divmod(k, KSEG)
                    if kk_ == 0:
                        G = Gs[gi % NSEG]
                        gi += 1
                        src = bass.AP(self.K2h[n][cc], ch * 2 * SEQ + 128 * KSEG * sidx, [[1, 128], [1, KSEG * 128]])
                        S.dma(C.q(), G[:, :], src, reads=["K2_%d_%d_%d" % (2 * SEQ, n, cc)], writes=[G.name])
                    m = 127 - k if n == 0 else k - 127
                    I0, I1 = max(0, m), min(127, 127 + m)
                    J0 = I0 - m
                    cnt = I1 - I0 + 1
                    if n == 0:
                        rhs = Zv[:, ch, 2 + J0:2 + J0 + cnt]
                        rtok = [Zv.name]
                    else:
                        rhs = z1t[:, J0:J0 + cnt]
                        rtok = [z1t.name]
                    S.op("tensor", lambda e, P=P, G=G, kk_=kk_, rhs=rhs, I0=I0, cnt=cnt, k=k: e.matmul(P[:, I0:I0 + cnt], lhsT=G[:, kk_ * 128:(kk_ + 1) * 128], rhs=rhs, start=(k == 0), stop=(k == 254)),
                         reads=[G.name] + rtok, writes=[P.name])
                if n == 0:
                    S.op("vector", lambda e, P=P, z1t=z1t, ch=ch: e.tensor_tensor(out=z1t[:], in0=P[:, 0:128], in1=Zx1[:, ch, 2:130], op=ALU.mult), reads=[P.name, Zx1.name], writes=[z1t.name])
                else:
                    S.op("vector", lambda e, P=P, ch=ch: e.tensor_tensor(out=Zv[:, ch, 2:130], in0=P[:, 0:128], in1=Zx2[:, ch, 2:130], op=ALU.mult), reads=[P.name, Zx2.name], writes=["Zvo%d" % ch])
                Pc = PS[4 + ((2 * ch + n) % 4)]
                gcv = Gc[:, n, ch % 8, :]
                rc = Zv[:, ch, 0:2] if n == 0 else z1ct[:, 0:2]
                rtok = [Zv.name] if n == 0 else [z1ct.name]
                plan = [(1, 0, 2, 0, 2)]
                if n == 0:
                    plan += [(0, 1, 2, 0, 1), (2, 0, 1, 1, 2)]
                else:
                    plan += [(0, 0, 1, 1, 2), (2, 1, 2, 0, 1)]
                for pi, (k, o0, o1, r0, r1) in enumerate(plan):
                    S.op("tensor", lambda e, Pc=Pc, gcv=gcv, rc=rc, k=k, o0=o0, o1=o1, r0=r0, r1=r1, pi=pi: e.matmul(Pc[:, o0:o1], lhsT=gcv[:, k * 128:(k + 1) * 128], rhs=rc[:, r0:r1], start=(pi == 0), stop=(pi == 2)),
                         reads=[Gc.name + "%d" % n] + rtok, writes=[Pc.name])
                if n == 0:
                    S.op("vector", lambda e, Pc=Pc, z1ct=z1ct, ch=ch: e.tensor_tensor(out=z1ct[:], in0=Pc[:, 0:2], in1=Zx1[:, ch, 0:2], op=ALU.mult), reads=[Pc.name, Zx1.name], writes=[z1ct.name])
                else:
                    S.op("vector", lambda e, Pc=Pc, ch=ch: e.tensor_tensor(out=Zv[:, ch, 0:2], in0=Pc[:, 0:2], in1=Zx2[:, ch, 0:2], op=ALU.mult), reads=[Pc.name, Zx2.name], writes=["Zvc%d" % ch])
        ob = [C.sb("hob%d" % i, [128, 512], BF16) for i in range(2)]
        alltok = ["Zvo%d" % ch for ch in range(128)] + ["Zvc%d" % ch for ch in range(128)]
        k = 0
        nt = 0
        for j in range(4):
            for ti in range(len(TILES)):
                t0, w = TILES[ti]
                p, co = self.tile_part(ti)
                o_ = ob[k % 2]
                k += 1
                for blk in range(w // 128):
                    Jz = blk if ti == 0 else 2 + 32 * j + (t0 - CTX) // 128 + blk
                    Pt = PS[nt % 4]
                    nt += 1
                    S.op("tensor", lambda e, Pt=Pt, Jz=Jz: e.matmul(Pt[:, 0:128], lhsT=Zv[:, :, Jz], rhs=self.ident_bf[:], start=True, stop=True), reads=alltok + [self.ident_bf.name], writes=[Pt.name])
                    S.op("scalar", lambda e, Pt=Pt, o_=o_, blk=blk: e.activation(out=o_[:, blk * 128:(blk + 1) * 128], in_=Pt[:, 0:128], func=AF.Copy), reads=[Pt.name], writes=[o_.name])
                S.dma(C.q(), self.os[cc][j][p][:, co:co + w], o_[:, :w], reads=[o_.name], writes=["os_%d_%d_%d_t%d" % (cc, j, p, ti)])

    def phase_B_deltanet(self, l, base_mark):
        C = self.C
        W = self.mix_w[l]
        NCH = (CTX + SEQ) // 128
        gT = C.sb("dn_g", [128, 4, NCH])
        bT = C.sb("dn_b", [128, 4, NCH])
        mk = C.mark()
        stage = 3
        self.dn_proj(l, W, gT, bT)
        C.release(mk)
        if stage >= 2:
            self.dn_scan(l, W, gT, bT)
            C.release(mk)
        if stage >= 3:
            self.dn_finish(l, W)
        C.release(base_mark)
        if stage >= 3:
            self.ag_outputs()

    def dn_proj(self, l, W, gT, bT):
        C, S = self.C, self.S
        PS = C.PS
        wq = C.sb("dwq", [128, KC, 1024], BF16)
        wab = C.sb("dwab", [128, KC, 8], BF16)
        stg = [C.sb("dstg%d" % i, [128, 2048]) for i in range(2)]
        cw = C.sb("dcw", [128, 6, 3])
        nea = C.sb("dnea", [128, 4])
        dtb = C.sb("ddtb", [128, 4])
        hst = [C.sb("dhh%d" % i, [128, KC, 512], BF16) for i in range(2)]
        X = [C.sb("dX%d" % i, [128, 512]) for i in range(2)]
        sqb = C.sb("dsqb", [128, 512], BF16)
        rn = C.sb("drn", [128, 512])
        Xb = [C.sb("dXb%d" % i, [128, 512], BF16) for i in range(3)]
        tk = [C.sb("dtk%d" % i, [128, 128], BF16) for i in range(3)]
        zt = [C.sb("dzt%d" % i, [128, 256], BF16) for i in range(2)]
        t4 = C.sb("dt4", [128, 4])
        si = [0]
        load_cast_weight(C, wq, wq.name, W["w_qkvz"], D, 1024, stg, si)
        sg = stg[si[0] % 2]
        si[0] += 1
        S.dma("sync", sg[:, :64].rearrange("p (c n) -> p c n", c=8), W["w_ab"].rearrange("(c p) n -> p c n", p=128), writes=[sg.name])
        S.op("gpsimd", lambda e: e.tensor_copy(out=wab[:], in_=sg[:, :64].rearrange("p (c n) -> p c n", c=8)), reads=[sg.name], writes=[wab.name])
        S.dma("sync", cw[:], W["conv"], writes=[cw.name])
        S.dma("sync", nea[:], W["alog_bc"], writes=[nea.name])
        S.dma("sync", dtb[:], W["dtb_bc"], writes=[dtb.name])
        S.op("scalar", lambda e: e.activation(out=nea[:], in_=nea[:], func=AF.Exp), reads=[nea.name], writes=[nea.name])
        S.op("vector", lambda e: e.tensor_scalar(out=nea[:], in0=nea[:], scalar1=-1.0, scalar2=None, op0=ALU.mult), reads=[nea.name], writes=[nea.name])
        k = 0
        cnt = 0
        for j in range(4):
            for ti in range(len(TILES)):
                if j > 0 and ti == 0:
                    continue
                t0, w = TILES[ti]
                p, co = self.tile_part(ti)
                s0 = 0 if ti == 0 else CTX + j * LQ + (t0 - CTX)
                hh_ = hst[k % 2]
                k += 1
                for c in range(KC):
                    S.dma(C.q(), hh_[:, c, :w], self.hall[c][p][j * 128:(j + 1) * 128, co:co + w], reads=["hall_%d_%d" % (c, p)], writes=[hh_.name + "c%d" % c])
                rowlen = w if ti == 0 else 64
                for kind in range(3):
                    for hd in range(2):
                        col = kind * 2 + hd
                        P = PS[cnt % 4]
                        Xk = X[cnt % 2]
                        xb = Xb[cnt % 3]
                        cnt += 1
                        for c in range(KC):
                            S.op("tensor", lambda e, P=P, c=c, col=col, hh_=hh_, w=w: e.matmul(P[:, :w], lhsT=wq[:, c, col * 128:(col + 1) * 128], rhs=hh_[:, c, :w], start=(c == 0), stop=(c == KC - 1)),
                                 reads=[wq.name, hh_.name + "c%d" % c], writes=[P.name])
                        Pv = P[:, :w].rearrange("p (r k) -> p r k", k=rowlen)
                        Xv = Xk[:, :w].rearrange("p (r k) -> p r k", k=rowlen)
                        S.op("vector", lambda e, Pv=Pv, Xv=Xv, col=col: e.tensor_scalar(out=Xv, in0=Pv, scalar1=cw[:, col, 1:2], scalar2=None, op0=ALU.mult), reads=[P.name, cw.name], writes=[Xk.name])
                        S.op("vector", lambda e, Pv=Pv, Xv=Xv, col=col, rowlen=rowlen: e.scalar_tensor_tensor(out=Xv[:, :, 1:rowlen], in0=Pv[:, :, 0:rowlen - 1], scalar=cw[:, col, 0:1], in1=Xv[:, :, 1:rowlen], op0=ALU.mult, op1=ALU.add),
                             reads=[P.name, cw.name, Xk.name], writes=[Xk.name])
                        S.op("vector", lambda e, Pv=Pv, Xv=Xv, col=col, rowlen=rowlen: e.scalar_tensor_tensor(out=Xv[:, :, 0:rowlen - 1], in0=Pv[:, :, 1:rowlen], scalar=cw[:, col, 2:3], in1=Xv[:, :, 0:rowlen - 1], op0=ALU.mult, op1=ALU.add),
                             reads=[P.name, cw.name, Xk.name], writes=[Xk.name])
                        S.op("scalar", lambda e, Xk=Xk, w=w: e.activation(out=Xk[:, :w], in_=Xk[:, :w], func=AF.Silu), reads=[Xk.name], writes=[Xk.name])
                        if kind < 2:
                            Pss = PS[4 + (cnt % 2)]
                            S.op("scalar", lambda e, Xk=Xk, w=w: e.activation(out=sqb[:, :w], in_=Xk[:, :w], func=AF.Square), reads=[Xk.name], writes=[sqb.name])
                            S.op("tensor", lambda e, Pss=Pss, w=w: e.matmul(Pss[:, :w], lhsT=self.ones_bf[:], rhs=sqb[:, :w], start=True, stop=True), reads=[sqb.name, self.ones_bf.name], writes=[Pss.name])
                            S.op("scalar", lambda e, Pss=Pss, w=w: e.activation(out=rn[:, :w], in_=Pss[:, :w], func=AF.Sqrt, bias=C.epsc[:, 0:1]), reads=[Pss.name], writes=[rn.name])
                            S.op("vector", lambda e, w=w: e.reciprocal(out=rn[:, :w], in_=rn[:, :w]), reads=[rn.name], writes=[rn.name])
                            scl = (128.0 ** -0.5) if kind == 0 else 1.0
                            S.op("vector", lambda e, Xk=Xk, xb=xb, w=w, scl=scl: e.scalar_tensor_tensor(out=xb[:, :w], in0=Xk[:, :w], scalar=scl, in1=rn[:, :w], op0=ALU.mult, op1=ALU.mult),
                                 reads=[Xk.name, rn.name], writes=[xb.name])
                            dst = self.dq[hd] if kind == 0 else self.dk[hd]
                            S.dma(C.q(), dst[:, s0:s0 + w], xb[:, :w], reads=[xb.name], writes=["d%s_%d_%d" % ("qk"[kind], hd, s0 // 128 + bb) for bb in range(w // 128)])
                        else:
                            S.op("gpsimd", lambda e, Xk=Xk, xb=xb, w=w: e.tensor_copy(out=xb[:, :w], in_=Xk[:, :w]), reads=[Xk.name], writes=[xb.name])
                        if kind >= 1:
                            dstt = self.dkt[hd] if kind == 1 else self.dvt[hd]
                            for blk in range(w // 128):
                                Pt = PS[6 + (cnt % 2)]
                                t_ = tk[cnt % 3]
                                cnt += 1
                                S.op("tensor", lambda e, Pt=Pt, xb=xb, blk=blk: e.matmul(Pt[:, 0:128], lhsT=xb[:, blk * 128:(blk + 1) * 128], rhs=self.ident_bf[:], start=True, stop=True),
                                     reads=[xb.name, self.ident_bf.name], writes=[Pt.name])
                                S.op("scalar", lambda e, Pt=Pt, t_=t_: e.activation(out=t_[:], in_=Pt[:, 0:128], func=AF.Copy), reads=[Pt.name], writes=[t_.name])
                                S.dma(C.q(), dstt[s0 + blk * 128:s0 + (blk + 1) * 128, :], t_[:], reads=[t_.name], writes=["d%st_%d_%d" % ("qkv"[kind], hd, s0 // 128 + blk)])
                for blk in range(w // 128):
                    Jz = s0 // 128 + blk
                    Pz = PS[cnt % 4]
                    z_ = zt[cnt % 2]
                    cnt += 1
                    for c in range(KC):
                        S.op("tensor", lambda e, Pz=Pz, c=c, hh_=hh_, blk=blk: e.matmul(Pz[:, 0:256], lhsT=hh_[:, c, blk * 128:(blk + 1) * 128], rhs=wq[:, c, 768:1024], start=(c == 0), stop=(c == KC - 1)),
                             reads=[wq.name, hh_.name + "c%d" % c], writes=[Pz.name])
                    S.op("scalar", lambda e, Pz=Pz, z_=z_: e.activation(out=z_[:], in_=Pz[:, 0:256], func=AF.Silu), reads=[Pz.name], writes=[z_.name])
                    S.dma(C.q(), self.dz[s0 + blk * 128:s0 + (blk + 1) * 128, :], z_[:], reads=[z_.name], writes=["dz_%d" % Jz])
                    Pab = PS[4 + (cnt % 2)]
                    for c in range(KC):
                        S.op("tensor", lambda e, Pab=Pab, c=c, hh_=hh_, blk=blk: e.matmul(Pab[:, 0:8], lhsT=hh_[:, c, blk * 128:(blk + 1) * 128], rhs=wab[:, c, :], start=(c == 0), stop=(c == KC - 1)),
                             reads=[wab.name, hh_.name + "c%d" % c], writes=[Pab.name])
                    S.op("vector", lambda e, Pab=Pab: e.tensor_tensor(out=t4[:], in0=Pab[:, 0:4], in1=dtb[:], op=ALU.add), reads=[Pab.name, dtb.name], writes=[t4.name])
                    S.op("scalar", lambda e: e.activation(out=t4[:], in_=t4[:], func=AF.Exp), reads=[t4.name], writes=[t4.name])
                    S.op("scalar", lambda e: e.activation(out=t4[:], in_=t4[:], func=AF.Ln, bias=self.ones_f[:, 0:1]), reads=[t4.name, self.ones_f.name], writes=[t4.name])
                    S.op("vector", lambda e, Jz=Jz: e.tensor_tensor(out=gT[:, :, Jz], in0=t4[:], in1=nea[:], op=ALU.mult), reads=[t4.name, nea.name], writes=[gT.name])
                    S.op("scalar", lambda e, Pab=Pab, Jz=Jz: e.activation(out=bT[:, :, Jz], in_=Pab[:, 4:8], func=AF.Sigmoid), reads=[t4.name], writes=[bT.name, Pab.name])

    def dn_scan(self, l, W, gT, bT):
        C, S = self.C, self.S
        PS = C.PS
        NCH = (CTX + SEQ) // 128
        msk = C.sb("dmsk", [128, 8, 128])
        S.dma("sync", msk[:], W["masks"], writes=[msk.name])
        bmk = C.sb("dbmk", [128, 5, 128])
        S.dma("sync", bmk[:], W["bmasks"], writes=[bmk.name])
        gc = C.sb("dgc", [128, 4, NCH])
        ngc = C.sb("dngc", [128, 4, NCH])
        egc = C.sb("degc", [128, 4, NCH])
        egl = C.sb("degl", [128, 4, NCH])
        ekd = C.sb("dekd", [128, 4, NCH])
        nb = C.sb("dnb", [128, 4, NCH])
        nbk = C.sb("dnbk", [128, 4, NCH])
        def cslot(ci, i):
            b = 2 * ci + i // 4
            return T(PS[b][:, (i % 4) * 128:(i % 4 + 1) * 128], "PSB%d" % b)

        class _SX:
            def op(self_, eng, fn, reads=(), writes=(), **kw):
                r = [t for t in reads if not t.startswith("PSB")]
                w = list(writes) + [t for t in reads if t.startswith("PSB")]
                return S.op(eng, fn, reads=r, writes=w, **kw)

            def dma(self_, *a, **kw):
                return S.dma(*a, **kw)
        SX = _SX()
        for d in range(2):
            Pc = T(PS[d].ap, "PSB%d" % d)
            Pl = T(PS[2 + d].ap, "PSB%d" % (2 + d))
            SX.op("tensor", lambda e, d=d, Pc=Pc: e.matmul(Pc[:, 0:2 * NCH], lhsT=msk[:, d, :], rhs=gT[:, 2 * d:2 * d + 2, :].rearrange("p a n -> p (a n)"), start=True, stop=True),
                 reads=[msk.name, gT.name], writes=[Pc.name])
            gcd = gc[:, 2 * d:2 * d + 2, :].rearrange("p a n -> p (a n)")
            SX.op("vector", lambda e, Pc=Pc, gcd=gcd: e.tensor_copy(out=gcd, in_=Pc[:, 0:2 * NCH]), reads=[Pc.name], writes=[gc.name + "%d" % d])
            SX.op("tensor", lambda e, d=d, Pl=Pl, gcd=gcd: e.matmul(Pl[:, 0:2 * NCH], lhsT=msk[:, 6 + d, :], rhs=gcd, start=True, stop=True), reads=[msk.name, gc.name + "%d" % d], writes=[Pl.name])
            egld = egl[:, 2 * d:2 * d + 2, :].rearrange("p a n -> p (a n)")
            ekdd = ekd[:, 2 * d:2 * d + 2, :].rearrange("p a n -> p (a n)")
            SX.op("scalar", lambda e, Pl=Pl, egld=egld: e.activation(out=egld, in_=Pl[:, 0:2 * NCH], func=AF.Exp), reads=[Pl.name], writes=[egl.name + "%d" % d])
            SX.op("vector", lambda e, Pl=Pl, ekdd=ekdd, gcd=gcd: e.tensor_tensor(out=ekdd, in0=Pl[:, 0:2 * NCH], in1=gcd, op=ALU.subtract), reads=[Pl.name, gc.name + "%d" % d], writes=[ekd.name + "%d" % d])
            SX.op("scalar", lambda e, ekdd=ekdd: e.activation(out=ekdd, in_=ekdd, func=AF.Exp), reads=[ekd.name + "%d" % d], writes=[ekd.name + "%d" % d])
        gtok = [gc.name + "0", gc.name + "1"]
        S.op("vector", lambda e: e.tensor_scalar(out=ngc[:], in0=gc[:], scalar1=-1.0, scalar2=None, op0=ALU.mult), reads=gtok, writes=[ngc.name])
        S.op("scalar", lambda e: e.activation(out=egc[:], in_=gc[:], func=AF.Exp), reads=gtok, writes=[egc.name])
        S.op("vector", lambda e: e.tensor_scalar(out=nb[:], in0=bT[:], scalar1=-1.0, scalar2=None, op0=ALU.mult), reads=[bT.name], writes=[nb.name])
        S.op("vector", lambda e: e.tensor_tensor(out=nbk[:], in0=nb[:], in1=ekd[:], op=ALU.mult), reads=[nb.name, ekd.name + "0", ekd.name + "1"], writes=[nbk.name])
        sc_tok = [ngc.name, egc.name, egl.name + "0", egl.name + "1", nb.name, nbk.name] + gtok


        chains = [(d, hd) for d in range(2) for hd in range(2)]
        GRP = 8
        grp = [[dict(q=C.sb("gq%d_%d" % (ci, b), [128, GRP * 128], BF16), k=C.sb("gk%d_%d" % (ci, b), [128, GRP * 128], BF16),
                     kt=C.sb("gkt%d_%d" % (ci, b), [128, GRP, 128], BF16), vt=C.sb("gvt%d_%d" % (ci, b), [128, GRP, 128], BF16)) for b in range(2)] for ci in range(4)]
        S32 = [C.sb("S32_%d" % ci, [128, 128]) for ci in range(4)]
        Sb = [C.sb("Sb_%d" % ci, [128, 128], BF16) for ci in range(4)]
        tmpf = [[{n_: C.sb("t%s%d_%d" % (n_, ci, b), [128, 128]) for n_ in ("D", "dec", "decs", "N32", "NT32", "P32", "PT32", "t1", "o")} for b in range(2)] for ci in range(4)]
        tmpb = [[{n_: C.sb("b%s%d_%d" % (n_, ci, b), [128, 128], BF16) for n_ in ("attn", "Nb", "Q0", "Q1", "QT0", "QT1", "Pb", "PTb", "Cb", "CTb", "Yb", "YTb", "rneg", "vn", "vn2")} for b in range(2)] for ci in range(4)]
        for ci in range(4):
            S.op("gpsimd", lambda e, ci=ci: e.memset(S32[ci][:], 0.0), writes=[S32[ci].name])
            S.op("gpsimd", lambda e, ci=ci: e.memset(Sb[ci][:], 0.0), writes=[Sb[ci].name])
        lat_groups = [list(range(2 + GRP * g, 2 + GRP * (g + 1))) for g in range((NCH - 2) // GRP)]
        order = {0: [[0, 1]] + lat_groups, 1: [[1, 0]] + [list(reversed(g)) for g in reversed(lat_groups)]}
        ngroups = len(order[0])
        ident_f, ident_bf, ones_f = self.ident, self.ident_bf, self.ones_f

        def load_group(ci, gi):
            d, hd = chains[ci]
            js = order[d][gi]
            j0, n = min(js), len(js)
            g = grp[ci][gi % 2]
            S.dma(C.q(), g["q"][:, :n * 128], self.dq[hd][:, j0 * 128:(j0 + n) * 128], reads=["dq_%d_%d" % (hd, bb) for bb in range(j0, j0 + n)], writes=[g["q"].name])
            S.dma(C.q(), g["k"][:, :n * 128], self.dk[hd][:, j0 * 128:(j0 + n) * 128], reads=["dk_%d_%d" % (hd, bb) for bb in range(j0, j0 + n)], writes=[g["k"].name])
            S.dma("sync", g["kt"][:, :n, :], self.dkt[hd][j0 * 128:(j0 + n) * 128, :].rearrange("(g t) d -> t g d", t=128), reads=["dkt_%d_%d" % (hd, bb) for bb in range(j0, j0 + n)], writes=[g["kt"].name])
            S.dma("sync", g["vt"][:, :n, :], self.dvt[hd][j0 * 128:(j0 + n) * 128, :].rearrange("(g t) d -> t g d", t=128), reads=["dvt_%d_%d" % (hd, bb) for bb in range(j0, j0 + n)], writes=[g["vt"].name])

        def pre_stages(ci, gi, jj, par):
            d, hd = chains[ci]
            js = order[d][gi]
            Jz = js[jj]
            loc = Jz - min(js)
            g = grp[ci][gi % 2]
            qT = g["q"][:, loc * 128:(loc + 1) * 128]
            kT = g["k"][:, loc * 128:(loc + 1) * 128]
            f, b = tmpf[ci][par], tmpb[ci][par]
            col = lambda a: a[:, ci, Jz:Jz + 1]
            st = []
            Pg, Pkk, Pkq = cslot(ci, 0), cslot(ci, 1), cslot(ci, 2)

            def s1():
                SX.op("vector", lambda e: e.tensor_scalar(out=f["D"][:], in0=ident_f[:], scalar1=col(gc), scalar2=None, op0=ALU.mult), reads=gtok + [ident_f.name], writes=[f["D"].name])
                SX.op("tensor", lambda e: e.matmul(Pg[:], lhsT=ones_f[:], rhs=f["D"][:], start=True, stop=False), reads=[f["D"].name, ones_f.name], writes=[Pg.name])
                SX.op("tensor", lambda e: e.matmul(Pg[:], lhsT=ident_f[:], rhs=msk[:, 2 + d, :], start=False, stop=True), reads=[msk.name, ident_f.name], writes=[Pg.name])
                SX.op("tensor", lambda e: e.matmul(Pkk[:], lhsT=kT, rhs=kT, start=True, stop=True), reads=[g["k"].name], writes=[Pkk.name])
                SX.op("tensor", lambda e: e.matmul(Pkq[:], lhsT=kT, rhs=qT, start=True, stop=True), reads=[g["k"].name, g["q"].name], writes=[Pkq.name])
            st.append(s1)

            def s2():
                SX.op("scalar", lambda e: e.activation(out=f["dec"][:], in_=Pg[:], func=AF.Exp, bias=col(ngc)), reads=[Pg.name, ngc.name], writes=[f["dec"].name])
                SX.op("gpsimd", lambda e: e.tensor_tensor(out=f["decs"][:], in0=f["dec"][:], in1=msk[:, 4 + d, :], op=ALU.mult), reads=[f["dec"].name, msk.name], writes=[f["decs"].name])
                SX.op("vector", lambda e: e.tensor_tensor(out=b["attn"][:], in0=Pkq[:], in1=f["dec"][:], op=ALU.mult), reads=[Pkq.name, f["dec"].name], writes=[b["attn"].name])
                SX.op("vector", lambda e: e.scalar_tensor_tensor(out=f["N32"][:], in0=Pkk[:], scalar=col(nb), in1=f["decs"][:], op0=ALU.mult, op1=ALU.mult), reads=[Pkk.name, nb.name, f["decs"].name], writes=[f["N32"].name])
                SX.op("gpsimd", lambda e: e.tensor_copy(out=b["Nb"][:], in_=f["N32"][:]), reads=[f["N32"].name], writes=[b["Nb"].name])
                SX.op("gpsimd", lambda e: e.tensor_tensor(out=b["Q0"][:], in0=f["N32"][:], in1=bmk[:, 0, :], op=ALU.mult), reads=[f["N32"].name, bmk.name], writes=[b["Q0"].name])
                SX.op("vector", lambda e: e.tensor_tensor(out=f["P32"][:], in0=b["Q0"][:], in1=ident_f[:], op=ALU.add), reads=[b["Q0"].name, ident_f.name], writes=[f["P32"].name])
                SX.op("scalar", lambda e: e.activation(out=b["Pb"][:], in_=f["P32"][:], func=AF.Copy), reads=[f["P32"].name], writes=[b["Pb"].name])
            st.append(s2)

            def s3():
                Pt = cslot(ci, 0)
                SX.op("tensor", lambda e: e.matmul(Pt[:], lhsT=b["Nb"][:], rhs=ident_bf[:], start=True, stop=True), reads=[b["Nb"].name, ident_bf.name], writes=[Pt.name])
                SX.op("scalar", lambda e: e.activation(out=f["NT32"][:], in_=Pt[:], func=AF.Copy), reads=[Pt.name], writes=[f["NT32"].name])
                SX.op("gpsimd", lambda e: e.tensor_tensor(out=b["QT0"][:], in0=f["NT32"][:], in1=bmk[:, 0, :], op=ALU.mult), reads=[f["NT32"].name, bmk.name], writes=[b["QT0"].name])
                SX.op("vector", lambda e: e.tensor_tensor(out=f["PT32"][:], in0=b["QT0"][:], in1=ident_f[:], op=ALU.add), reads=[b["QT0"].name, ident_f.name], writes=[f["PT32"].name])
                SX.op("scalar", lambda e: e.activation(out=b["PTb"][:], in_=f["PT32"][:], func=AF.Copy), reads=[f["PT32"].name], writes=[b["PTb"].name])
            st.append(s3)

            def upd(Pa, Pb_):
                SX.op("vector", lambda e: e.tensor_tensor(out=f["P32"][:], in0=f["P32"][:], in1=Pa[:], op=ALU.add), reads=[Pa.name, f["P32"].name], writes=[f["P32"].name])
                SX.op("vector", lambda e: e.tensor_tensor(out=f["PT32"][:], in0=f["PT32"][:], in1=Pb_[:], op=ALU.add), reads=[Pb_.name, f["PT32"].name], writes=[f["PT32"].name])
                SX.op("gpsimd", lambda e: e.tensor_copy(out=b["Pb"][:], in_=f["P32"][:]), reads=[f["P32"].name], writes=[b["Pb"].name])
                SX.op("scalar", lambda e: e.activation(out=b["PTb"][:], in_=f["PT32"][:], func=AF.Copy), reads=[f["PT32"].name], writes=[b["PTb"].name])

            for lv in range(1, 3):
                def slv(lv=lv):
                    qo, qn = "Q%d" % ((lv - 1) % 2), "Q%d" % (lv % 2)
                    to, tn = "QT%d" % ((lv - 1) % 2), "QT%d" % (lv % 2)
                    Pqt, Pq = cslot(ci, 0), cslot(ci, 1)
                    SX.op("tensor", lambda e: e.matmul(Pqt[:], lhsT=b[qo][:], rhs=b[to][:], start=True, stop=True), reads=[b[qo].name, b[to].name], writes=[Pqt.name])
                    SX.op("tensor", lambda e: e.matmul(Pq[:], lhsT=b[to][:], rhs=b[qo][:], start=True, stop=True), reads=[b[qo].name, b[to].name], writes=[Pq.name])
                    SX.op("scalar", lambda e: e.activation(out=b[tn][:], in_=Pqt[:], func=AF.Copy), reads=[Pqt.name], writes=[b[tn].name])
                    SX.op("vector", lambda e: e.tensor_copy(out=b[qn][:], in_=Pq[:]), reads=[Pq.name], writes=[b[qn].name])
                    Pa, Pb_ = cslot(ci, 2), cslot(ci, 0)
                    SX.op("tensor", lambda e: e.matmul(Pa[:], lhsT=b[tn][:], rhs=b["Pb"][:], start=True, stop=True), reads=[b[tn].name, b["Pb"].name], writes=[Pa.name])
                    SX.op("tensor", lambda e: e.matmul(Pb_[:], lhsT=b["Pb"][:], rhs=b[tn][:], start=True, stop=True), reads=[b[tn].name, b["Pb"].name], writes=[Pb_.name])
                    upd(Pa, Pb_)
                st.append(slv)
            for mi in range(4):
                def smg(mi=mi):
                    SX.op("gpsimd", lambda e: e.tensor_tensor(out=b["Cb"][:], in0=f["N32"][:], in1=bmk[:, 1 + mi, :], op=ALU.mult), reads=[f["N32"].name, bmk.name], writes=[b["Cb"].name])
                    SX.op("gpsimd", lambda e: e.tensor_tensor(out=b["CTb"][:], in0=f["NT32"][:], in1=bmk[:, 1 + mi, :], op=ALU.mult), reads=[f["NT32"].name, bmk.name], writes=[b["CTb"].name])
                    Py, Pyt = cslot(ci, 0), cslot(ci, 1)
                    SX.op("tensor", lambda e: e.matmul(Py[:], lhsT=b["CTb"][:], rhs=b["Pb"][:], start=True, stop=True), reads=[b["CTb"].name, b["Pb"].name], writes=[Py.name])
                    SX.op("tensor", lambda e: e.matmul(Pyt[:], lhsT=b["Cb"][:], rhs=b["PTb"][:], start=True, stop=True), reads=[b["Cb"].name, b["PTb"].name], writes=[Pyt.name])
                    SX.op("scalar", lambda e: e.activation(out=b["Yb"][:], in_=Py[:], func=AF.Copy), reads=[Py.name], writes=[b["Yb"].name])
                    SX.op("vector", lambda e: e.tensor_copy(out=b["YTb"][:], in_=Pyt[:]), reads=[Pyt.name], writes=[b["YTb"].name])
                    Pa, Pb_ = cslot(ci, 2), cslot(ci, 0)
                    SX.op("tensor", lambda e: e.matmul(Pa[:], lhsT=b["PTb"][:], rhs=b["Yb"][:], start=True, stop=True), reads=[b["PTb"].name, b["Yb"].name], writes=[Pa.name])
                    SX.op("tensor", lambda e: e.matmul(Pb_[:], lhsT=b["Pb"][:], rhs=b["YTb"][:], start=True, stop=True), reads=[b["Pb"].name, b["YTb"].name], writes=[Pb_.name])
                    upd(Pa, Pb_)
                st.append(smg)
            return st

        def scan_stages(ci, gi, jj, par):
            d, hd = chains[ci]
            js = order[d][gi]
            Jz = js[jj]
            loc = Jz - min(js)
            g = grp[ci][gi % 2]
            qT = g["q"][:, loc * 128:(loc + 1) * 128]
            kT = g["k"][:, loc * 128:(loc + 1) * 128]
            ktm = g["kt"][:, loc, :]
            vtm = g["vt"][:, loc, :]
            f, b = tmpf[ci][par], tmpb[ci][par]
            col = lambda a: a[:, ci, Jz:Jz + 1]
            dst = (self.dof if d == 0 else self.dob)[hd]
            R1, O1, R2, O2, Sd = cslot(ci, 3), cslot(ci, 4), cslot(ci, 5), cslot(ci, 6), cslot(ci, 7)
            st = []

            def a1():
                SX.op("tensor", lambda e: e.matmul(R1[:], lhsT=kT, rhs=Sb[ci][:], start=True, stop=True), reads=[g["k"].name, Sb[ci].name], writes=[R1.name])
                SX.op("tensor", lambda e: e.matmul(O1[:], lhsT=qT, rhs=Sb[ci][:], start=True, stop=True), reads=[g["q"].name, Sb[ci].name], writes=[O1.name])
                SX.op("vector", lambda e: e.scalar_tensor_tensor(out=b["rneg"][:], in0=R1[:], scalar=col(egc), in1=vtm, op0=ALU.mult, op1=ALU.subtract), reads=[R1.name, egc.name, g["vt"].name], writes=[b["rneg"].name])
                SX.op("scalar", lambda e: e.activation(out=f["t1"][:], in_=O1[:], func=AF.Identity, scale=col(egc)), reads=[O1.name, egc.name], writes=[f["t1"].name])
            st.append(a1)

            def a2():
                SX.op("tensor", lambda e: e.matmul(R2[:], lhsT=b["Pb"][:], rhs=b["rneg"][:], start=True, stop=True), reads=[b["Pb"].name, b["rneg"].name], writes=[R2.name])
                SX.op("scalar", lambda e: e.activation(out=b["vn"][:], in_=R2[:], func=AF.Identity, scale=col(nb)), reads=[R2.name, nb.name], writes=[b["vn"].name])
                SX.op("vector", lambda e: e.tensor_scalar(out=b["vn2"][:], in0=R2[:], scalar1=col(nbk), scalar2=None, op0=ALU.mult), reads=[R2.name, nbk.name], writes=[b["vn2"].name])
            st.append(a2)

            def a3():
                SX.op("tensor", lambda e: e.matmul(Sd[:], lhsT=ktm, rhs=b["vn2"][:], start=True, stop=True), reads=[g["kt"].name, b["vn2"].name], writes=[Sd.name])
                SX.op("tensor", lambda e: e.matmul(O2[:], lhsT=b["attn"][:], rhs=b["vn"][:], start=True, stop=True), reads=[b["attn"].name, b["vn"].name], writes=[O2.name])
                SX.op("vector", lambda e: e.scalar_tensor_tensor(out=S32[ci][:], in0=S32[ci][:], scalar=col(egl), in1=Sd[:], op0=ALU.mult, op1=ALU.add), reads=[Sd.name, S32[ci].name, egl.name + "%d" % d], writes=[S32[ci].name])
                SX.op("scalar", lambda e: e.activation(out=Sb[ci][:], in_=S32[ci][:], func=AF.Copy), reads=[S32[ci].name], writes=[Sb[ci].name])
                SX.op("vector", lambda e: e.tensor_tensor(out=f["o"][:], in0=O2[:], in1=f["t1"][:], op=ALU.add), reads=[O2.name, f["t1"].name], writes=[f["o"].name])
                S.dma(C.q(), dst[Jz * 128:(Jz + 1) * 128, :], f["o"][:], reads=[f["o"].name], writes=["do%s_%d_%d" % ("fb"[d], hd, Jz)])
            st.append(a3)
            return st

        steps = [(gi, jj) for gi in range(ngroups) for jj in range(len(order[0][gi]))]
        for ci in range(4):
            load_group(ci, 0)
        pre = {ci: pre_stages(ci, steps[0][0], steps[0][1], 0) for ci in range(4)}
        for stg_i in range(len(pre[0])):
            for ci in range(4):
                pre[ci][stg_i]()
        for t, (gi, jj) in enumerate(steps):
            par = t % 2
            nxt = steps[t + 1] if t + 1 < len(steps) else None
            if nxt is not None and nxt[1] == 0:
                for ci in range(4):
                    load_group(ci, nxt[0])
            sc = {ci: scan_stages(ci, gi, jj, par) for ci in range(4)}
            pr = {ci: pre_stages(ci, nxt[0], nxt[1], 1 - par) for ci in range(4)} if nxt is not None else None
            npre = len(pr[0]) if pr else 0
            pi = 0
            for si_ in range(3):
                for ci in range(4):
                    sc[ci][si_]()
                for _ in range(3):
                    if pi < npre:
                        for ci in range(4):
                            pr[ci][pi]()
                        pi += 1
            while pi < npre:
                for ci in range(4):
                    pr[ci][pi]()
                pi += 1

    def dn_finish(self, l, W):
        C, S = self.C, self.S
        PS = C.PS
        onb = C.sb("donb", [128, 128])
        S.dma("sync", onb[:], W["onorm_bc"], writes=[onb.name])
        of = [C.sb("dof%d" % i, [128, 4, 128]) for i in range(2)]
        ob_ = [C.sb("dob%d" % i, [128, 4, 128]) for i in range(2)]
        zz = [C.sb("dzz%d" % i, [128, 4, 256], BF16) for i in range(2)]
        ssq = C.sb("dssq", [128, 4])
        junk = C.sb("djunk", [128, 4, 128])
        og = [C.sb("dog%d" % i, [128, 128], BF16) for i in range(3)]
        osb = [[C.sb("dosb%d_%d" % (hd, i), [128, 512], BF16) for i in range(2)] for hd in range(2)]
        k = 0
        cnt = 0
        for j in range(4):
            for ti in range(len(TILES)):
                t0, w = TILES[ti]
                p, co = self.tile_part(ti)
                s0 = 0 if ti == 0 else CTX + j * LQ + (t0 - CTX)
                nb_ = w // 128
                zt_ = zz[k % 2]
                S.dma("sync", zt_[:, :nb_, :], self.dz[s0:s0 + w, :].rearrange("(g t) d -> t g d", t=128), reads=["dz_%d" % (s0 // 128 + bb) for bb in range(nb_)], writes=[zt_.name])
                for hd in range(2):
                    f_, b_ = of[(k + hd) % 2], ob_[(k + hd) % 2]
                    o_ = osb[hd][k % 2]
                    S.dma("sync", f_[:, :nb_, :], self.dof[hd][s0:s0 + w, :].rearrange("(g t) d -> t g d", t=128), reads=["dof_%d_%d" % (hd, s0 // 128 + bb) for bb in range(nb_)], writes=[f_.name])
                    S.dma("sync", b_[:, :nb_, :], self.dob[hd][s0:s0 + w, :].rearrange("(g t) d -> t g d", t=128), reads=["dob_%d_%d" % (hd, s0 // 128 + bb) for bb in range(nb_)], writes=[b_.name])
                    S.op("vector", lambda e, f_=f_, b_=b_, nb_=nb_: e.tensor_tensor(out=f_[:, :nb_, :], in0=f_[:, :nb_, :], in1=b_[:, :nb_, :], op=ALU.add), reads=[f_.name, b_.name], writes=[f_.name])
                    S.op("gpsimd", lambda e, f_=f_, nb_=nb_: e.tensor_tensor(out=junk[:, :nb_, :], in0=f_[:, :nb_, :], in1=f_[:, :nb_, :], op=ALU.mult), reads=[f_.name], writes=[junk.name])
                    S.op("vector", lambda e, nb_=nb_: e.tensor_reduce(out=ssq[:, :nb_], in_=junk[:, :nb_, :], axis=AX.X, op=ALU.add), reads=[junk.name], writes=[ssq.name])
                    sst = [ssq.name]
                    S.op("scalar", lambda e, nb_=nb_: e.activation(out=ssq[:, :nb_], in_=ssq[:, :nb_], func=AF.Sqrt, scale=1.0 / 128.0, bias=C.epsc[:, 0:1]), reads=sst, writes=[ssq.name])
                    S.op("vector", lambda e, nb_=nb_: e.reciprocal(out=ssq[:, :nb_], in_=ssq[:, :nb_]), reads=[ssq.name], writes=[ssq.name])
                    for blk in range(nb_):
                        g_ = og[cnt % 3]
                        Pt = PS[cnt % 4]
                        cnt += 1
                        S.op("vector", lambda e, f_=f_, blk=blk: e.scalar_tensor_tensor(out=f_[:, blk, :], in0=f_[:, blk, :], scalar=ssq[:, blk:blk + 1], in1=onb[:], op0=ALU.mult, op1=ALU.mult),
                             reads=[f_.name, ssq.name, onb.name], writes=[f_.name])
                        S.op("gpsimd", lambda e, f_=f_, blk=blk, g_=g_, zt_=zt_, hd=hd: e.tensor_tensor(out=g_[:], in0=f_[:, blk, :], in1=zt_[:, blk, hd * 128:(hd + 1) * 128], op=ALU.mult),
                             reads=[f_.name, zt_.name], writes=[g_.name])
                        S.op("tensor", lambda e, Pt=Pt, g_=g_: e.matmul(Pt[:, 0:128], lhsT=g_[:], rhs=self.ident_bf[:], start=True, stop=True), reads=[g_.name, self.ident_bf.name], writes=[Pt.name])
                        S.op("scalar", lambda e, Pt=Pt, o_=o_, blk=blk: e.activation(out=o_[:, blk * 128:(blk + 1) * 128], in_=Pt[:, 0:128], func=AF.Copy), reads=[Pt.name], writes=[o_.name])
                    S.dma(C.q(), self.os[hd][j][p][:, co:co + w], o_[:, :w], reads=[o_.name], writes=["os_%d_%d_%d_t%d" % (hd, j, p, ti)])
                k += 1

    def phase_shortconv(self, l):
        C, S = self.C, self.S
        PS = C.PS
        W = self.mix_w[l]
        base = l * 48
        self.set_AB(l, base + 8, base + 0)
        w_in = C.sb("scwin", [128, KC, 3 * D], BF16)
        w_out = C.sb("scwout", [128, KC, D], BF16)
        cw = C.sb("sccw", [128, KC, 3])
        stg = [C.sb("scstg%d" % i, [128, 2048]) for i in range(2)]
        xt = C.sb("scx", [128, KC, 512])
        sq = C.sb("scsq", [128, KC, 512], BF16)
        rstd = C.sb("scr", [128, 512])
        h = C.sb("sch", [128, KC, 512], BF16)
        bg = C.sb("scbg", [128, KC, 512])
        u = C.sb("scu", [128, KC, 514])
        cv = C.sb("sccv", [128, KC, 512])
        m = C.sb("scm", [128, KC, 512], BF16)
        si = [0]
        S.dma("sync", cw[:], W["conv"], writes=[cw.name])
        for cb in range(6):
            for c in range(KC):
                sg = stg[si[0] % 2]
                si[0] += 1
                S.dma(C.q(), sg[:, :512], W["w_in"][c * 128:(c + 1) * 128, cb * 512:(cb + 1) * 512], writes=[sg.name])
                S.op("gpsimd", lambda e, sg=sg, c=c, cb=cb: e.tensor_copy(out=w_in[:, c, cb * 512:(cb + 1) * 512], in_=sg[:, :512]), reads=[sg.name], writes=[w_in.name])
        for c in range(KC):
            sg = stg[si[0] % 2]
            si[0] += 1
            S.dma(C.q(), sg[:, :1024], W["w_out"][c * 128:(c + 1) * 128, :], writes=[sg.name])
            S.op("gpsimd", lambda e, sg=sg, c=c: e.tensor_copy(out=w_out[:, c, :], in_=sg[:, :1024]), reads=[sg.name], writes=[w_out.name])
        for ti in range(len(TILES)):
            t0, w = TILES[ti]
            which = 1 if ti == 0 else 0
            rowlen = w if ti == 0 else 64
            nrow = w // rowlen
            self.load_x(xt, ti)
            emit_norm_mod(C, xt, w, self.AB[:, which, 0, :], self.AB[:, which, 1, :], self.ones_bf, sq, PS[0], rstd, dst=h)
            for oc in range(KC):
                Pb, Pc, Px = PS[1 + (oc % 2)], PS[3 + (oc % 2)], PS[5 + (oc % 2)]
                for (P, colc) in ((Pb, oc), (Pc, 8 + oc), (Px, 16 + oc)):
                    for c in range(KC):
                        S.op("tensor", lambda e, P=P, colc=colc, c=c: e.matmul(P[:, :w], lhsT=w_in[:, c, colc * 128:(colc + 1) * 128], rhs=h[:, c, :w], start=(c == 0), stop=(c == KC - 1)),
                             reads=[w_in.name, h.name + "f%d" % c], writes=[P.name])
                S.op("scalar", lambda e, oc=oc, Pb=Pb: e.activation(out=bg[:, oc, :w], in_=Pb[:, :w], func=AF.Copy), reads=[Pb.name], writes=[bg.name + "%d" % oc])
                S.op("scalar", lambda e, oc=oc, Pc=Pc: e.activation(out=cv[:, oc, :w], in_=Pc[:, :w], func=AF.Copy), reads=[Pc.name], writes=[cv.name + "t%d" % oc])
                S.op("vector", lambda e, oc=oc, Px=Px: e.tensor_tensor(out=u[:, oc, :w], in0=cv[:, oc, :w], in1=Px[:, :w], op=ALU.mult), reads=[Px.name, cv.name + "t%d" % oc], writes=[u.name + "%d" % oc])
                uv = u[:, oc, :w].rearrange("p (r k) -> p r k", k=rowlen)
                cvv = cv[:, oc, :w].rearrange("p (r k) -> p r k", k=rowlen)
                S.op("vector", lambda e, oc=oc, uv=uv, cvv=cvv: e.tensor_scalar(out=cvv, in0=uv, scalar1=cw[:, oc, 1:2], scalar2=None, op0=ALU.mult),
                     reads=[u.name + "%d" % oc, cw.name, cv.name + "t%d" % oc], writes=[cv.name + "a%d" % oc])
                S.op("vector", lambda e, oc=oc, uv=uv, cvv=cvv: e.scalar_tensor_tensor(out=cvv[:, :, 1:rowlen], in0=uv[:, :, 0:rowlen - 1], scalar=cw[:, oc, 0:1], in1=cvv[:, :, 1:rowlen], op0=ALU.mult, op1=ALU.add),
                     reads=[u.name + "%d" % oc, cw.name, cv.name + "a%d" % oc], writes=[cv.name + "b%d" % oc])
                S.op("vector", lambda e, oc=oc, uv=uv, cvv=cvv: e.scalar_tensor_tensor(out=cvv[:, :, 0:rowlen - 1], in0=uv[:, :, 1:rowlen], scalar=cw[:, oc, 2:3], in1=cvv[:, :, 0:rowlen - 1], op0=ALU.mult, op1=ALU.add),
                     reads=[u.name + "%d" % oc, cw.name, cv.name + "b%d" % oc], writes=[cv.name + "c%d" % oc])
                S.op("gpsimd", lambda e, oc=oc: e.tensor_tensor(out=m[:, oc, :w], in0=cv[:, oc, :w], in1=bg[:, oc, :w], op=ALU.mult),
                     reads=[cv.name + "c%d" % oc, bg.name + "%d" % oc], writes=[m.name + "%d" % oc])
            for dc in range(KC):
                P = PS[7] if dc % 2 else PS[0]
                for c in range(KC):
                    S.op("tensor", lambda e, P=P, dc=dc, c=c: e.matmul(P[:, :w], lhsT=w_out[:, c, dc * 128:(dc + 1) * 128], rhs=m[:, c, :w], start=(c == 0), stop=(c == KC - 1)),
                         reads=[w_out.name, m.name + "%d" % c], writes=[P.name])
                S.op("vector", lambda e, P=P, dc=dc, which=which: e.scalar_tensor_tensor(out=xt[:, dc, :w], in0=P[:, :w], scalar=self.mod[:, base + 16 + dc, which:which + 1], in1=xt[:, dc, :w], op0=ALU.mult, op1=ALU.add),
                     reads=[P.name, self.mod.name, xt.name + "c%d" % dc, h.name + "n%d" % dc], writes=[xt.name + "f%d" % dc])
            self.store_x(xt, ti)

    def phase_moe(self, l):
        C, S = self.C, self.S
        PS = C.PS
        base = l * 48
        self.set_AB(4 + l, base + 32, base + 24)
        xt = C.sb("mx", [128, KC, 512])
        sq = C.sb("msq", [128, KC, 512], BF16)
        rstd = C.sb("mr", [128, 512])
        fT = C.sb("fT", [128, KC, 1536], BF16)
        acc = C.sb("acc", [128, 12, D])
        gate_all = C.sb("gate_all", [128, 12, NE])
        wb = [C.sb("wb%d" % i, [128, 3, 4096], BF16) for i in range(2)]
        hid = [C.sb("hid%d" % i, [128, 4, 512], BF16) for i in range(2)]
        st_ = [C.sb("st%d" % i, [128, 512]) for i in range(2)]
        tmp = (T(C.sb("sg", [128, NE]).ap, "sg"), T(C.sb("ch", [128, NE]).ap, "ch"), C.sb("psum", [128, 8, 6]), C.sb("pmin", [128, 8, 6]),
               C.sb("gs", [128, 8]), C.sb("m2", [128, 8]), C.sb("gmax", [128, 1]), C.sb("og", [128, 8]),
               C.sb("sel", [128, 8, 4]), C.sb("wsum", [128, 1]))
        widx = 0
        passes = PASSES if l != 3 else [[1, 2], [3, 4, 5], [6, 7, 8]]
        for tiles in passes:
            off = 0
            offs = []
            for ti in tiles:
                t0, w = TILES[ti]
                which = 1 if ti == 0 else 0
                self.load_x(xt, ti)
                emit_norm_mod(C, xt, w, self.AB[:, which, 0, :], self.AB[:, which, 1, :], self.ones_bf, sq, PS[0], rstd, out_bf=fT, out_off=off)
                emit_router(C, xt, w, self.wr, self.rb, PS[1], gate_all, off // 128, tmp)
                offs.append((ti, off, w))
                off += w
            items = [(ex, ti, off, w) for ex in range(NE) for (ti, off, w) in offs]
            wviews = {}

            def gu(item):
                ex, ti, off, w = item
                if ex not in wviews:
                    wt = wb[(widx0 + ex) % 2]
                    cc_, el = ex // 4, ex % 4
                    pb, qq = cc_ // 4, cc_ % 4
                    for m3 in range(3):
                        p = el * 3 + m3
                        r0 = pb * 256 + (qq % 2) * 128
                        S.dma(C.q(), wt[:, m3, :], self.wfull[l][p][qq // 2][r0:r0 + 128, :], reads=["wf%d_%d_%d" % (l, p, qq // 2)], writes=[wt.name + "m%d" % m3])
                    wviews[ex] = (wt, wt[:, 0, :].rearrange("p (c n) -> p c n", c=8), wt[:, 1, :].rearrange("p (c n) -> p c n", c=8), wt[:, 2, :].rearrange("p (c n) -> p c n", c=4))
                wt, wgv, wuv, wdv = wviews[ex]
                hb = hid[(ti + ex) % 2]
                for hc in range(4):
                    Pg = PS[2 + (hc % 2)]
                    Pu = PS[4 + (hc % 2)]
                    for c in range(KC):
                        S.op("tensor", lambda e, Pg=Pg, c=c, hc=hc, off=off, w=w, wgv=wgv: e.matmul(Pg[:, :w], lhsT=wgv[:, c, hc * 128:(hc + 1) * 128], rhs=fT[:, c, off:off + w], start=(c == 0), stop=(c == KC - 1)),
                             reads=[wt.name + "m0", fT.name + "c%d" % c], writes=[Pg.name])
                    for c in range(KC):
                        S.op("tensor", lambda e, Pu=Pu, c=c, hc=hc, off=off, w=w, wuv=wuv: e.matmul(Pu[:, :w], lhsT=wuv[:, c, hc * 128:(hc + 1) * 128], rhs=fT[:, c, off:off + w], start=(c == 0), stop=(c == KC - 1)),
                             reads=[wt.name + "m1", fT.name + "c%d" % c], writes=[Pu.name])
                    sb_ = st_[hc % 2]
                    S.op("scalar", lambda e, Pg=Pg, sb_=sb_, w=w: e.activation(out=sb_[:, :w], in_=Pg[:, :w], func=AF.Silu), reads=[Pg.name], writes=[sb_.name])
                    S.op("vector", lambda e, Pu=Pu, sb_=sb_, hb=hb, hc=hc, w=w: e.tensor_tensor(out=hb[:, hc, :w], in0=sb_[:, :w], in1=Pu[:, :w], op=ALU.mult),
                         reads=[Pu.name, sb_.name], writes=[hb.name + "h%d" % hc])

            def yacc(item):
                ex, ti, off, w = item
                wt, wgv, wuv, wdv = wviews[ex]
                hb = hid[(ti + ex) % 2]
                for s_ in range(w // 128):
                    sub = off // 128 + s_
                    for dh in range(2):
                        Py = PS[6 + ((s_ * 2 + dh) % 2)]
                        for hc in range(4):
                            S.op("tensor", lambda e, Py=Py, hb=hb, hc=hc, s_=s_, dh=dh, wdv=wdv: e.matmul(Py[:, :], lhsT=hb[:, hc, s_ * 128:(s_ + 1) * 128], rhs=wdv[:, hc, dh * 512:(dh + 1) * 512], start=(hc == 0), stop=(hc == 3)),
                                 reads=[hb.name + "h%d" % hc, wt.name + "m2"], writes=[Py.name])
                        an = "acc%d_%d" % (sub, dh)
                        if ex == 0:
                            S.op("vector", lambda e, Py=Py, sub=sub, dh=dh, ex=ex: e.tensor_scalar(out=acc[:, sub, dh * 512:(dh + 1) * 512], in0=Py[:, :], scalar1=gate_all[:, sub, ex:ex + 1], scalar2=None, op0=ALU.mult),
                                 reads=[Py.name, gate_all.name], writes=[an])
                        else:
                            S.op("vector", lambda e, Py=Py, sub=sub, dh=dh, ex=ex: e.scalar_tensor_tensor(out=acc[:, sub, dh * 512:(dh + 1) * 512], in0=Py[:, :], scalar=gate_all[:, sub, ex:ex + 1],
                                                                                                     in1=acc[:, sub, dh * 512:(dh + 1) * 512], op0=ALU.mult, op1=ALU.add),
                                 reads=[Py.name, gate_all.name, an], writes=[an])

            widx0 = widx
            for i, item in enumerate(items):
                gu(item)
                if i > 0:
                    yacc(items[i - 1])
            yacc(items[-1])
            widx += NE
            for (ti, off, w) in offs:
                which = 1 if ti == 0 else 0
                self.load_x(xt, ti)
                for c in range(KC):
                    Pt = PS[c % 2]
                    for s in range(w // 128):
                        sub = off // 128 + s
                        S.op("tensor", lambda e, Pt=Pt, s=s, sub=sub, c=c: e.transpose(out=Pt[:, s * 128:(s + 1) * 128], in_=acc[:, sub, c * 128:(c + 1) * 128], identity=self.ident[:]),
                             reads=["acc%d_%d" % (sub, c // 4), self.ident.name], writes=[Pt.name])
                    S.op("vector", lambda e, Pt=Pt, c=c, w=w, which=which: e.scalar_tensor_tensor(out=xt[:, c, :w], in0=Pt[:, :w], scalar=self.mod[:, base + 40 + c, which:which + 1], in1=xt[:, c, :w], op0=ALU.mult, op1=ALU.add),
                         reads=[Pt.name, self.mod.name, xt.name + "c%d" % c], writes=[xt.name + "f%d" % c])
                self.store_x(xt, ti)

    def phase_out(self):
        C, S = self.C, self.S
        PS = C.PS
        xt = C.sb("ox", [128, KC, 512])
        if not self.final_norm:
            for ti in range(len(TILES)):
                t0, w = TILES[ti]
                self.load_x(xt, ti)
                S.dma("gpsimd", fm(self.xo)[:, :, t0:t0 + w], xt[:, :, :w], reads=[xt.name + "c%d" % c for c in range(KC)], final=True)
            return
        sq = C.sb("osq", [128, KC, 512], BF16)
        rstd = C.sb("or", [128, 512])
        for ti in range(len(TILES)):
            t0, w = TILES[ti]
            self.load_x(xt, ti)
            xin = [xt.name + "c%d" % c for c in range(KC)]
            S.op("scalar", lambda e, w=w: e.activation(out=sq[:, :, :w], in_=xt[:, :, :w], func=AF.Square), reads=xin, writes=[sq.name])
            for c in range(KC):
                S.op("tensor", lambda e, c=c, w=w: e.matmul(PS[0][:, :w], lhsT=self.ones_bf[:], rhs=sq[:, c, :w], start=(c == 0), stop=(c == KC - 1)),
                     reads=[sq.name, self.ones_bf.name], writes=[PS[0].name])
            S.op("scalar", lambda e, w=w: e.activation(out=rstd[:, :w], in_=PS[0][:, :w], func=AF.Sqrt, scale=1.0 / D, bias=C.epsc[:, 0:1]), reads=[PS[0].name], writes=[rstd.name])
            S.op("vector", lambda e, w=w: e.reciprocal(out=rstd[:, :w], in_=rstd[:, :w]), reads=[rstd.name], writes=[rstd.name])
            for c in range(KC):
                S.op("vector", lambda e, c=c, w=w: e.scalar_tensor_tensor(out=xt[:, c, :w], in0=xt[:, c, :w], scalar=self.gain[:, 8, c:c + 1], in1=rstd[:, :w], op0=ALU.mult, op1=ALU.mult),
                     reads=[xt.name + "c%d" % c, rstd.name, self.gain.name], writes=[xt.name + "f%d" % c])
            S.dma("gpsimd", fm(self.xo)[:, :, t0:t0 + w], xt[:, :, :w], reads=[xt.name + "f%d" % c for c in range(KC)], final=True)

    def finish(self):
        return self.C.finish()


def _fmaj(v):
    v = np.asarray(v, np.float32)
    lead = v.shape[:-1]
    return np.ascontiguousarray(np.moveaxis(v.reshape(lead + (KC, 128)), -1, 0))


def host_inputs(inp, layers, x_state=None, ctx_state=None, do_moe=True, do_mixer=True):
    x = inp["x"] if x_state is None else x_state
    ctx = inp["ctx"] if ctx_state is None else ctx_state
    maps = []
    gains = np.concatenate([inp["norm_mix"], inp["norm_ffn"], inp["norm_final"][None]], axis=0)
    gains_fm = np.ascontiguousarray(_fmaj(gains))
    ada_bT = np.ascontiguousarray(inp["ada_b"].reshape(4 * 48, 128).T)
    ident = np.eye(128, dtype=np.float32)
    for c in range(NCORES):
        b, q = c // 4, c % 4
        xc = np.concatenate([ctx[b], x[b, q * LQ:(q + 1) * LQ]], axis=0)
        m = {
            "xT": np.ascontiguousarray(xc.T),
            "cT": np.ascontiguousarray(np.stack([inp["c"][0, c * 128:(c + 1) * 128], inp["c"][1, c * 128:(c + 1) * 128], inp["c_ctx"][c * 128:(c + 1) * 128]], axis=1)),
            "ada_w": np.ascontiguousarray(inp["ada_w"][:, c * 128:(c + 1) * 128, :]),
            "ada_bT": ada_bT,
            "bsel": np.ascontiguousarray(np.tile(np.eye(2, dtype=np.float32)[b][None], (128, 1))),
            "rsel": np.ascontiguousarray(np.tile(np.eye(4, dtype=np.float32)[q][None], (128, 1))),
            "gains": gains_fm,
            "w_router": inp["w_router"],
            "rbias": inp["router_bias"],
            "ident": ident,
        }
        if do_moe:
            for l in layers:
                m["wg%d" % l] = np.ascontiguousarray(inp["moe_w_gate"][l, 4 * c:4 * c + 4])
                m["wu%d" % l] = np.ascontiguousarray(inp["moe_w_up"][l, 4 * c:4 * c + 4])
                m["wd%d" % l] = np.ascontiguousarray(inp["moe_w_down"][l, 4 * c:4 * c + 4])
        if do_mixer:
            for l in layers:
                if l == 2:
                    m["sc_w_in"] = inp["sc_w_in"][0]
                    m["sc_convT"] = np.ascontiguousarray(_fmaj(inp["sc_conv"][0]))
                    m["sc_convT"] = np.ascontiguousarray(m["sc_convT"].transpose(0, 2, 1))
                    m["w_out2"] = inp["sc_w_out"][0]
            for l in layers:
                if do_mixer and l in (0, 3):
                    jd = 0 if l == 0 else 1
                    r = q
                    wi = inp["dn_w_in"][jd]
                    cols = []
                    for kind in range(4):
                        for hd in range(2):
                            head = 2 * r + hd
                            cols.append(wi[:, kind * D + head * 128:kind * D + (head + 1) * 128])
                    m["dn_w_qkvz%d" % l] = np.ascontiguousarray(np.concatenate(cols, axis=1))
                    abc = []
                    for ab in range(2):
                        for dd in range(2):
                            for hd in range(2):
                                abc.append(wi[:, 4 * D + ab * 16 + dd * 8 + 2 * r + hd][:, None])
                    m["dn_w_ab%d" % l] = np.ascontiguousarray(np.concatenate(abc, axis=1))
                    cv = inp["dn_conv"][jd]
                    cvc = []
                    for kind in range(3):
                        for hd in range(2):
                            head = 2 * r + hd
                            cvc.append(cv[:, kind * D + head * 128:kind * D + (head + 1) * 128])
                    m["dn_convT%d" % l] = np.ascontiguousarray(np.stack(cvc, axis=0).transpose(2, 0, 1))
                    al = np.array([inp["dn_a_log"][jd][dd, 2 * r + hd] for dd in range(2) for hd in range(2)], np.float32)
                    db = np.array([inp["dn_dt_bias"][jd][dd, 2 * r + hd] for dd in range(2) for hd in range(2)], np.float32)
                    m["dn_alog%d" % l] = np.ascontiguousarray(np.tile(al[None], (128, 1)))
                    m["dn_dtb%d" % l] = np.ascontiguousarray(np.tile(db[None], (128, 1)))
                    m["dn_masks%d" % l] = _DN_MASKS
                    m["dn_bmasks%d" % l] = _DN_BMASKS
                    m["dn_onorm%d" % l] = np.ascontiguousarray(np.tile(inp["dn_out_norm"][jd][None], (128, 1)))
                    m["w_out%d" % l] = inp["dn_w_out"][jd]
            if do_mixer and l_has(layers, 1):
                r = q
                chs = slice(256 * r, 256 * r + 256)
                wi = inp["hy_w_in"][0]
                m["hy_w_in"] = np.ascontiguousarray(np.concatenate([wi[:, 0:D][:, chs], wi[:, D:2 * D][:, chs], wi[:, 2 * D:3 * D][:, chs]], axis=1))
                cv = inp["hy_conv"][0]
                cvm = np.concatenate([cv[:, 0:D][:, chs], cv[:, D:2 * D][:, chs], cv[:, 2 * D:3 * D][:, chs]], axis=1)
                m["hy_convT"] = np.ascontiguousarray(cvm.reshape(3, 6, 128).transpose(2, 1, 0))
                m["w_out1"] = inp["hy_w_out"][0]
                m["hy_f_w1"] = inp["hy_f_w1"][0]
                m["hy_f_b1"] = np.ascontiguousarray(inp["hy_f_b1"][0][:, None])
                m["hy_f_fr"] = np.ascontiguousarray(inp["hy_f_freq"][0][:, None])
                m["hy_f_w2"] = inp["hy_f_w2"][0]
                m["hy_f_b2"] = np.ascontiguousarray(inp["hy_f_b2"][0][:, None])
                w3 = inp["hy_f_w3"][0].reshape(64, 2, 2, D)[:, :, :, chs]
                m["hy_f_w3"] = np.ascontiguousarray(w3.reshape(64, 8, 128))
                hb = inp["hy_bias"][0][:, chs]
                m["hy_biasT"] = np.ascontiguousarray(hb.reshape(2, 2, 128).transpose(2, 0, 1))
                m["hy_ndelta"] = np.ascontiguousarray(-_HY_DELTAS[chs].reshape(2, 128).T)
                m["hy_feat_lat"] = _hy_feat(SEQ)
                m["hy_feat_ctx"] = _hy_feat(CTX)
            if any(l in (0, 1, 3) for l in layers) and do_mixer:
                m["anti"] = np.ascontiguousarray(np.eye(128, dtype=np.float32)[::-1])
        maps.append(m)
    return maps


def l_has(layers, l):
    return l in layers


def _dn_masks():
    k = np.arange(128)[:, None]
    i = np.arange(128)[None, :]
    U_f = (k <= i); U_b = (k >= i)
    NEG_f = np.where(k <= i, 0.0, -30000.0); NEG_b = np.where(k >= i, 0.0, -30000.0)
    ST_f = (k < i); ST_b = (k > i)
    E_f = np.broadcast_to(k == 127, (128, 128)); E_b = np.broadcast_to(k == 0, (128, 128))
    return np.ascontiguousarray(np.stack([U_f, U_b, NEG_f, NEG_b, ST_f, ST_b, E_f, E_b], axis=1).astype(np.float32))


def _dn_bmasks():
    k = np.arange(128)[:, None]
    i = np.arange(128)[None, :]
    bd = lambda b: (k // b == i // b)
    ms = [bd(8)] + [bd(2 * b) & ~bd(b) for b in (8, 16, 32, 64)]
    return np.ascontiguousarray(np.stack(ms, axis=1).astype(np.float32))


_DN_MASKS = _dn_masks()
_DN_BMASKS = _dn_bmasks()
_HY_DELTAS = np.abs(np.linspace(math.log(1e-2) / 1.5, math.log(1e-2) / 0.3, D, dtype=np.float32))
_FEAT_CACHE = {}


def _hy_feat(l):
    if l not in _FEAT_CACHE:
        y = np.arange(2 * l, dtype=np.int64)
        pos = np.abs(y - (l - 1)).astype(np.float32)[:, None]
        t = pos / np.float32(max(l - 1, 1))
        bands = np.linspace(1e-4, 16 - 1, 16, dtype=np.float32)[None, :]
        ang = np.float32(2 * math.pi / l) * pos * bands
        feat = np.concatenate([t, np.cos(ang), -np.sin(ang)], axis=-1).astype(np.float32)
        _FEAT_CACHE[l] = np.ascontiguousarray(feat.T)
    return _FEAT_CACHE[l]


def host_gather(results):
    x = np.empty((2, SEQ, D), np.float32)
    ctx = np.empty((2, CTX, D), np.float32)
    for c in range(NCORES):
        b, q = c // 4, c % 4
        xo = results[c]["xo"]
        x[b, q * LQ:(q + 1) * LQ] = xo[:, CTX:].T
        if q == 0:
            ctx[b] = xo[:, :CTX].T
    return x, ctx


def kernel(**inputs):
    inp = {k: np.asarray(v) for k, v in inputs.items()}
    P = Prog(layers=(0, 1, 2, 3), final_norm=True)
    nc, info = P.finish()
    maps = host_inputs(inp, [0, 1, 2, 3])
    res = run_bass_kernel_spmd(nc, maps, core_ids=list(range(NCORES)))
    x, _ = host_gather(res.results)
    return x
```
